# Optimizing a Trainium2 kernel written in Bass

```python
import jax, jax.numpy as jnp
from jax import lax
import numpy as np

D_MODEL = 1024
BATCH = 8
SEQ = 4096
DEPTH = 2

D_FF = 2816
CHUNK = 128
A_GROUPS = 4
A_GROUP_CH = 128
A_HALF = A_GROUPS * A_GROUP_CH
N_HEADS = 8
HEAD_DIM = 64
ATT_W = N_HEADS * HEAD_DIM
IDX_HEADS = 8
IDX_DIM = 64
TOPK_MAX = 256
Q_BLOCK = 128
ROPE_THETA = 10000.0
EPS = 1e-6

SPLIT_SIZES = (A_HALF, A_HALF, ATT_W, ATT_W, ATT_W, IDX_HEADS * IDX_DIM, IDX_DIM, IDX_HEADS, D_MODEL, D_MODEL)
SPLIT_POINTS = tuple(int(v) for v in np.cumsum(SPLIT_SIZES)[:-1])
N_IN = int(sum(SPLIT_SIZES))

kernel_name = "hybrid_gmlp_dsa_macaron"


def _rms_norm(x, g):
    xf = x.astype(jnp.float32)
    y = xf * lax.rsqrt(jnp.mean(xf * xf, axis=-1, keepdims=True) + EPS)
    return (y * g.astype(jnp.float32)).astype(x.dtype)


def _layer_norm(x, g, b):
    xf = x.astype(jnp.float32)
    mu = jnp.mean(xf, axis=-1, keepdims=True)
    var = jnp.mean(jnp.square(xf - mu), axis=-1, keepdims=True)
    y = (xf - mu) * lax.rsqrt(var + EPS)
    return (y * g.astype(jnp.float32) + b.astype(jnp.float32)).astype(x.dtype)


def _swiglu(h, w_gu, w_down):
    gate, up = jnp.split(h @ w_gu, 2, axis=-1)
    return (jax.nn.silu(gate) * up) @ w_down


def _rope(x, pos):
    d = x.shape[-1]
    inv = ROPE_THETA ** (-jnp.arange(0, d, 2, dtype=jnp.float32) / d)
    ang = pos.astype(jnp.float32)[:, None] * inv[None, :]
    cos = jnp.cos(ang)[None, :, None, :].astype(x.dtype)
    sin = jnp.sin(ang)[None, :, None, :].astype(x.dtype)
    x1, x2 = x[..., : d // 2], x[..., d // 2:]
    return jnp.concatenate([x1 * cos - x2 * sin, x2 * cos + x1 * sin], axis=-1)


def _gmlp_spatial(u, v, ln_g, ln_b, w_s, b_s):
    B, T, _ = u.shape
    u = jax.nn.gelu(u)
    v = _layer_norm(jax.nn.gelu(v), ln_g, ln_b)
    vb = v.reshape(B, T // CHUNK, CHUNK, A_GROUPS, A_GROUP_CH)
    mask = jnp.tril(jnp.ones((CHUNK, CHUNK), dtype=bool))
    ws = jnp.where(mask[None], w_s, jnp.zeros((), w_s.dtype))
    mixed = jnp.einsum('gts,bnsgc->bntgc', ws, vb) + b_s.T[:, :, None]
    return u * mixed.reshape(B, T, A_HALF)


def _dsa_attention(q, k, v, q_idx, k_idx, w_idx):
    B, T, H, Dh = q.shape
    L = k.shape[1]
    topk = min(TOPK_MAX, L // 4)
    nb = T // Q_BLOCK
    idx_scale = (IDX_DIM ** -0.5) * (IDX_HEADS ** -0.5)
    att_scale = HEAD_DIM ** -0.5
    kpos = jnp.arange(L, dtype=jnp.int32)
    qpos = jnp.arange(T, dtype=jnp.int32).reshape(nb, Q_BLOCK)
    gather = jax.vmap(lambda a, i: a[i])

    def to_blocks(a):
        return a.reshape((B, nb, Q_BLOCK) + a.shape[2:]).swapaxes(0, 1)

    def one_block(args):
        qb, qib, wb, pb = args
        logits = jnp.einsum('bqhd,bsd->bqhs', qib, k_idx).astype(jnp.float32)
        score = jnp.einsum('bqhs,bqh->bqs', jax.nn.relu(logits), wb.astype(jnp.float32)) * idx_scale
        causal = kpos[None, :] <= pb[:, None]
        score = jnp.where(causal[None], score, -jnp.inf)
        _, sel = lax.top_k(score, topk)
        k_sel = gather(k, sel)
        v_sel = gather(v, sel)
        s = jnp.einsum('bqhd,bqkhd->bhqk', qb, k_sel).astype(jnp.float32) * att_scale
        valid = sel <= pb[None, :, None]
        s = jnp.where(valid[:, None], s, -jnp.inf)
        p = jax.nn.softmax(s, axis=-1).astype(v.dtype)
        return jnp.einsum('bhqk,bqkhd->bqhd', p, v_sel)

    out = lax.map(one_block, (to_blocks(q), to_blocks(q_idx), to_blocks(w_idx), qpos))
    return out.swapaxes(0, 1).reshape(B, T, H * Dh)


def _hybrid_mixer(h, w_in, b_gate, gmlp_ln_g, gmlp_ln_b, gmlp_w_s, gmlp_b_s, w_pa, w_pb, w_out):
    B, T, _ = h.shape
    proj = h @ w_in
    u, va, q, k, v, qi, ki, wi, g_a, g_b = jnp.split(proj, SPLIT_POINTS, axis=-1)
    pos = jnp.arange(T, dtype=jnp.int32)
    y_a = _gmlp_spatial(u, va, gmlp_ln_g, gmlp_ln_b, gmlp_w_s, gmlp_b_s) @ w_pa
    q = _rope(q.reshape(B, T, N_HEADS, HEAD_DIM), pos)
    k = _rope(k.reshape(B, T, N_HEADS, HEAD_DIM), pos)
    v = v.reshape(B, T, N_HEADS, HEAD_DIM)
    qi = _rope(qi.reshape(B, T, IDX_HEADS, IDX_DIM), pos)
    ki = _rope(ki[:, :, None, :], pos)[:, :, 0, :]
    y_b = _dsa_attention(q, k, v, qi, ki, wi) @ w_pb
    gates = jax.nn.sigmoid(jnp.concatenate([g_a, g_b], axis=-1) + b_gate)
    gate_a, gate_b = jnp.split(gates, 2, axis=-1)
    return (gate_a * y_a + gate_b * y_b) @ w_out


def setup_inputs(seed: int = 0) -> dict:
    key = jax.random.key(seed)
    ks = jax.random.split(key, 20)
    f32 = jnp.float32

    def nrm(k, shape, scale):
        return jax.random.normal(k, shape, f32) * scale

    def gain(k, shape):
        return 1.0 + 0.05 * jax.random.normal(k, shape, f32)

    return {
        "x": jax.random.normal(ks[0], (BATCH, SEQ, D_MODEL), f32),
        "ffn1_norm": gain(ks[1], (DEPTH, D_MODEL)),
        "ffn1_w_gu": nrm(ks[2], (DEPTH, D_MODEL, 2 * D_FF), D_MODEL ** -0.5),
        "ffn1_w_down": nrm(ks[3], (DEPTH, D_FF, D_MODEL), D_FF ** -0.5),
        "mix_norm": gain(ks[4], (DEPTH, D_MODEL)),
        "w_in": nrm(ks[5], (DEPTH, D_MODEL, N_IN), D_MODEL ** -0.5),
        "b_gate": nrm(ks[6], (DEPTH, 2 * D_MODEL), 0.02),
        "gmlp_ln_g": gain(ks[7], (DEPTH, A_HALF)),
        "gmlp_ln_b": nrm(ks[8], (DEPTH, A_HALF), 0.02),
        "gmlp_w_s": nrm(ks[9], (DEPTH, A_GROUPS, CHUNK, CHUNK), 0.5 * CHUNK ** -0.5),
        "gmlp_b_s": 1.0 + 0.1 * jax.random.normal(ks[10], (DEPTH, A_GROUPS, CHUNK), f32),
        "w_pa": nrm(ks[11], (DEPTH, A_HALF, D_MODEL), A_HALF ** -0.5),
        "w_pb": nrm(ks[12], (DEPTH, ATT_W, D_MODEL), ATT_W ** -0.5),
        "w_out": nrm(ks[13], (DEPTH, D_MODEL, D_MODEL), D_MODEL ** -0.5),
        "ffn2_norm": gain(ks[14], (DEPTH, D_MODEL)),
        "ffn2_w_gu": nrm(ks[15], (DEPTH, D_MODEL, 2 * D_FF), D_MODEL ** -0.5),
        "ffn2_w_down": nrm(ks[16], (DEPTH, D_FF, D_MODEL), D_FF ** -0.5),
        "final_norm": gain(ks[17], (D_MODEL,)),
    }


def reference(x, ffn1_norm, ffn1_w_gu, ffn1_w_down, mix_norm, w_in, b_gate, gmlp_ln_g, gmlp_ln_b,
              gmlp_w_s, gmlp_b_s, w_pa, w_pb, w_out, ffn2_norm, ffn2_w_gu, ffn2_w_down, final_norm):
    for l in range(DEPTH):
        x = x + 0.5 * _swiglu(_rms_norm(x, ffn1_norm[l]), ffn1_w_gu[l], ffn1_w_down[l])
        x = x + _hybrid_mixer(_rms_norm(x, mix_norm[l]), w_in[l], b_gate[l], gmlp_ln_g[l], gmlp_ln_b[l],
                              gmlp_w_s[l], gmlp_b_s[l], w_pa[l], w_pb[l], w_out[l])
        x = x + 0.5 * _swiglu(_rms_norm(x, ffn2_norm[l]), ffn2_w_gu[l], ffn2_w_down[l])
    return _rms_norm(x, final_norm)
```

```python
import numpy as np
import ml_dtypes
from contextlib import ExitStack
import concourse.bass as bass
import concourse.mybir as mybir
from concourse.bass_utils import run_bass_kernel_spmd

F32 = mybir.dt.float32
BF16 = mybir.dt.bfloat16
U32 = mybir.dt.uint32
AF = mybir.ActivationFunctionType
ALU = mybir.AluOpType
AX = mybir.AxisListType

D = 1024
KD = D // 128
NHEAD = 8
HDIM = 64
TOPK_MAX = 256
EPS = 1e-6
IDX_SCALE = (64 ** -0.5) * (8 ** -0.5)
ATT_SCALE = 64 ** -0.5
NEG = -30000.0
WA_COLS = 3848
WB_COLS = 3072


class Res:
    __slots__ = ("name", "w", "rs")

    def __init__(self, name):
        self.name = name
        self.w = None
        self.rs = []


class Eng:
    def __init__(self, name, sem):
        self.name = name
        self.sem = sem
        self.count = 0
        self.ops = []
        self.known = {}


class Prog:
    def __init__(self, nc, stack):
        self.nc = nc
        self.stack = stack
        self.eng = {}
        self.nsem = 0
        for n in ("pe", "act", "dve", "pool", "sp"):
            self.eng[n] = Eng(n, self.new_sem("sem_" + n))
        self._dsem = {}
        self._dpool = {}
        self._dtoks = []
        self.nblocks = 0
        self.ninst = 0

    def new_sem(self, name):
        s = self.stack.enter_context(self.nc.semaphore(name))
        self.nsem += 1
        return s

    def _collect(self, e, reads, writes, is_dma):
        waits = {}

        def add(tok, same_ok):
            if tok is None:
                return
            sem, val, en = tok
            if en == e.name and same_ok:
                return
            if e.known.get(id(sem), 0) >= val:
                return
            k = id(sem)
            if k not in waits or waits[k][1] < val:
                waits[k] = tok
        pe = (e.name == "pe") and not is_dma
        for r in reads:
            add(r.w, pe)
        for r in writes:
            add(r.w, pe)
            for t in r.rs:
                add(t, pe)
        for t in waits.values():
            e.known[id(t[0])] = max(e.known.get(id(t[0]), 0), t[1])
        return list(waits.values())

    def op(self, en, fn, reads=(), writes=(), signal=True):
        e = self.eng[en]
        waits = self._collect(e, reads, writes, False)
        if signal:
            e.count += 1
            tok = (e.sem, e.count, en)
            e.ops.append((waits, fn, (e.sem, 1)))
        else:
            tok = (e.sem, e.count + 1, en)
            e.ops.append((waits, fn, None))
        for r in reads:
            r.rs.append(tok)
        for r in writes:
            r.w = tok
            r.rs = []
        return tok

    def dma(self, en, pairs, reads=(), writes=(), semres=None):
        e = self.eng[en]
        key = id(semres)
        if key not in self._dsem:
            pool_ = self._dpool.setdefault(en, [])
            if pool_:
                ent = pool_.pop()
                ent[2] = Res("ser")
            else:
                ent = [self.new_sem("dsem%d" % self.nsem), 0, Res("ser"), en]
            self._dsem[key] = ent
        ent = self._dsem[key]
        reads = list(reads)
        writes = list(writes) + [ent[2]]
        waits = self._collect(e, reads, writes, True)
        ent[1] += 16 * len(pairs)
        tok = (ent[0], ent[1], None)
        for i, (o, a) in enumerate(pairs):
            e.ops.append((waits if i == 0 else [],
                          (lambda h, o=o, a=a: h.dma_start(out=o, in_=a)), (ent[0], 16)))
        for r in reads:
            r.rs.append(tok)
        for r in writes:
            r.w = tok
            r.rs = []
        return tok

    def end_phase(self):
        sp = self.eng["sp"]
        for ent in self._dsem.values():
            if ent[1] > 0:
                sp.ops.append(([(ent[0], ent[1], None)], None, None))
        for ent in self._dsem.values():
            self._dpool[ent[3]].append(ent)
        self._dsem = {}
        self.emit()

    def emit(self):
        nc = self.nc
        handles = {"pe": "tensor", "act": "scalar", "dve": "vector", "pool": "gpsimd", "sp": "sync"}
        with nc.Block() as block:
            for en, attr in handles.items():
                e = self.eng[en]

                def body(h, e=e):
                    for waits, fn, inc in e.ops:
                        for (sem, val, _) in waits:
                            h.wait_ge(sem, val)
                        if fn is not None:
                            ins = fn(h)
                            if inc is not None:
                                ins.then_inc(inc[0], inc[1])
                getattr(block, attr)(body)
                self.ninst += len(e.ops)
                e.ops = []
        self.nblocks += 1


def build(T, DEPTH, DFF, NIT=15, stop_after=None):
    NB = T // 128
    NTL = T // 512
    NF = DFF // 128
    topk = min(TOPK_MAX, T // 4)
    nc = bass.Bass("TRN2", target_bir_lowering=False)

    def din(name, shape, dt=F32):
        return nc.dram_tensor(name, list(shape), dt, kind="ExternalInput").ap()

    x_d = din("x", [T, D])
    nrm_d = din("nrm", [DEPTH * 3 + 1, D])
    wgu_d = [din("wgu1", [DEPTH, D, 2 * DFF]), din("wgu2", [DEPTH, D, 2 * DFF])]
    wdn_d = [din("wdn1", [DEPTH, DFF, D]), din("wdn2", [DEPTH, DFF, D])]
    wA_d = din("wA", [DEPTH, D, WA_COLS])
    wB_d = din("wB", [DEPTH, D, WB_COLS])
    bgT_d = din("bgT", [DEPTH, 128, 16])
    lng_d = din("lng", [DEPTH, 512])
    lnb_d = din("lnb", [DEPTH, 512])
    wsT_d = din("wsT", [DEPTH, 128, 512])
    bsb_d = din("bsb", [DEPTH, 512])
    wpa_d = din("wpa", [DEPTH, 512, D])
    wpb_d = din("wpb", [DEPTH, 512, D])
    wout_d = din("wout", [DEPTH, D, D])
    ident_d = din("ident", [128, 128], BF16)
    cos_d = din("cosT", [128, T])
    sin_d = din("sinT", [128, T])
    dmask_d = din("dmask", [128, 128])
    tril_d = din("tril", [128, 512])
    pwt_d = din("pwt", [128, 48])
    out_d = nc.dram_tensor("out", [T, D], F32, kind="ExternalOutput").ap()
    xs_d = nc.dram_tensor("xs", [T, D], F32, kind="Internal").ap()
    qT_d = nc.dram_tensor("qT_s", [128, 4, T], BF16, kind="Internal").ap()
    qiT_d = nc.dram_tensor("qiT_s", [128, 4, T], BF16, kind="Internal").ap()
    oT_d = nc.dram_tensor("oT_s", [128, 4, T], BF16, kind="Internal").ap()
    r_xs = [Res("xs%d" % b) for b in range(NB)]
    r_qTd = [Res("qTd%d" % i) for i in range(NTL)]
    r_qiTd = [Res("qiTd%d" % i) for i in range(NTL)]
    r_oTd = [Res("oTd%d" % b) for b in range(NB)]
    r_out = Res("out")

    with ExitStack() as gst:
        P = Prog(nc, gst)

        uid = [0]

        def sb(st, name, shape, dt):
            uid[0] += 1
            return st.enter_context(nc.sbuf_tensor("%s_u%d" % (name, uid[0]), list(shape), dt))

        def ps(name, shape, dt):
            return gst.enter_context(nc.psum_tensor(name, list(shape), dt))

        pA = [ps("pA%d" % i, [128, 512], F32) for i in range(2)]
        pB = [ps("pB%d" % i, [128, 512], F32) for i in range(2)]
        pD = [ps("pD%d" % i, [128, 512], F32) for i in range(2)]
        pT = ps("pT0", [128, 1024], BF16)
        pX = ps("pX0", [128, 512], F32)
        r_pA = [Res("pA0"), Res("pA1")]
        r_pB = [Res("pB0"), Res("pB1")]
        r_pD = [Res("pD0"), Res("pD1")]
        r_pT = Res("pT")
        r_pX = Res("pX")
        ident = sb(gst, "ident_sb", [128, 128], BF16)
        ss_all = sb(gst, "ss_all", [128, NB], F32)
        rstd_all = sb(gst, "rstd_all", [128, NB], F32)
        r_ident, r_ss, r_rstd = Res("ident"), Res("ss"), Res("rstd")

        def mm(out, lhsT, rhs, start, stop, reads, writes, signal=None):
            P.op("pe", lambda h: h.matmul(out, lhsT=lhsT, rhs=rhs, start=start, stop=stop), reads, writes,
                 signal=(stop if signal is None else signal))

        def tr(out, in_, reads, writes, signal=True):
            P.op("pe", lambda h: h.transpose(out=out, in_=in_, identity=ident[:, :]), list(reads) + [r_ident], writes,
                 signal=signal)

        def act(out, in_, func, reads, writes, scale=1.0, bias=None, accum=None):
            kw = {}
            if bias is not None:
                kw["bias"] = bias
            if accum is not None:
                kw["accum_out"] = accum
            P.op("act", lambda h: h.activation(out=out, in_=in_, func=func, scale=scale, **kw), reads, writes)

        def ts(en, out, in0, s1, op0, reads, writes, s2=None, op1=None, accum=None):
            def f(h):
                kw = {}
                if accum is not None:
                    kw["accum_out"] = accum
                if op1 is None:
                    return h.tensor_scalar(out=out, in0=in0, scalar1=s1, scalar2=None, op0=op0, **kw)
                return h.tensor_scalar(out=out, in0=in0, scalar1=s1, scalar2=s2, op0=op0, op1=op1, **kw)
            P.op(en, f, reads, writes)

        def tt(en, out, in0, in1, op, reads, writes):
            P.op(en, lambda h: h.tensor_tensor(out=out, in0=in0, in1=in1, op=op), reads, writes)

        def stt(out, in0, scalar, in1, op0, op1, reads, writes, accum=None):
            if accum is None:
                P.op("dve", lambda h: h.scalar_tensor_tensor(out=out, in0=in0, scalar=scalar, in1=in1,
                                                             op0=op0, op1=op1), reads, writes)
            else:
                P.op("dve", lambda h: h.scalar_tensor_tensor(out=out, in0=in0, scalar=scalar, in1=in1,
                                                             op0=op0, op1=op1, accum_out=accum), reads, writes)

        def cp(en, out, in_, reads, writes):
            P.op(en, lambda h: h.tensor_copy(out=out, in_=in_), reads, writes)

        P.dma("sp", [(ident[:, :], ident_d)], writes=[r_ident], semres=r_ident)

        def load_w(st, name, src2d, kch, cols, csplit, eng="pool"):
            t = sb(st, name, [128, kch, cols], BF16)
            v = src2d.rearrange("(k p) f -> p k f", p=128)
            rs = []
            for c0 in range(0, cols, csplit):
                c1 = min(cols, c0 + csplit)
                r = Res("%s_%d" % (name, c0))
                P.dma(eng, [(t[:, :, c0:c1], v[:, :, c0:c1])], writes=[r], semres=r)
                rs.append(r)
            return t, rs, csplit

        def compute_rstd():
            act(rstd_all[:, :], ss_all[:, :], AF.Sqrt, [r_ss], [r_rstd], scale=1.0 / D, bias=EPS)
            P.op("dve", lambda h: h.reciprocal(out=rstd_all[:, :], in_=rstd_all[:, :]), [r_rstd], [r_rstd])

        class NormCtx:
            def __init__(self, st, g_row, xsrc):
                self.xsrc = xsrc
                self.gb = sb(st, "gb", [128, D], F32)
                self.r_gb = Res("gb")
                P.dma("sp", [(self.gb[:, :], nrm_d[g_row:g_row + 1, :].broadcast_to([128, D]))],
                      writes=[self.r_gb], semres=self.r_gb)
                self.xin = [sb(st, "xin%d" % i, [128, D], F32) for i in range(2)]
                self.r_xin = [Res("xin0"), Res("xin1")]
                self.hb = [sb(st, "hb%d" % i, [128, D], BF16) for i in range(2)]
                self.r_hb = [Res("hb0"), Res("hb1")]
                self.hT = sb(st, "hT", [128, KD, 512], BF16)
                self.r_hT = [Res("hT%d" % s) for s in range(4)]
                self.n = 0

            def tile(self, i, src_res=None):
                for s in range(4):
                    blk = 4 * i + s
                    sl = self.n % 2
                    self.n += 1
                    rd = [src_res[blk]] if src_res is not None else []
                    P.dma("sp", [(self.xin[sl][:, :], self.xsrc[blk * 128:(blk + 1) * 128, :])],
                          reads=rd, writes=[self.r_xin[sl]], semres=self.r_xin[sl])
                    stt(self.hb[sl][:, :], self.xin[sl][:, :], rstd_all[:, blk:blk + 1], self.gb[:, :],
                        ALU.mult, ALU.mult, [self.r_xin[sl], r_rstd, self.r_gb], [self.r_hb[sl]])
                    for k in range(KD):
                        tr(pT[:, k * 128:(k + 1) * 128], self.hb[sl][:, k * 128:(k + 1) * 128],
                           [self.r_hb[sl]], [r_pT], signal=(k == KD - 1))
                    act(self.hT[:, :, s * 128:(s + 1) * 128], pT[:, :].rearrange("p (k t) -> p k t", k=KD),
                        AF.Copy, [r_pT], [self.r_hT[s]])

        if True:
            xin = [sb(gst, "pxin%d" % i, [128, D], F32) for i in range(2)]
            r_xin = [Res("pxin0"), Res("pxin1")]
            junk = sb(gst, "pjunk", [128, D], BF16)
            r_junk = Res("pjunk")

            def prologue_stats():
                for blk in range(NB):
                    sl = blk % 2
                    P.dma("sp", [(xin[sl][:, :], x_d[blk * 128:(blk + 1) * 128, :])], writes=[r_xin[sl]], semres=r_xin[sl])
                    stt(junk[:, :], xin[sl][:, :], 1.0, xin[sl][:, :], ALU.mult, ALU.mult,
                        [r_xin[sl]], [r_junk, r_ss], accum=ss_all[:, blk:blk + 1])

        def ffn_phase(l, which, xsrc, src_res, first=False):
            with ExitStack() as st:
                g_row = l * 3 + (0 if which == 0 else 2)
                wgu, wgu_r, gsp = load_w(st, "wgu", wgu_d[which][l], KD, 2 * DFF, 512)
                wdn, wdn_r, dsp = load_w(st, "wdn", wdn_d[which][l], NF, D, 512)
                if first:
                    prologue_stats()
                compute_rstd()
                ncx = NormCtx(st, g_row, xsrc)
                actT = sb(st, "actT", [128, NF, 512], BF16)
                r_actT = [Res("actT%d" % f) for f in range(NF)]
                sg = [sb(st, "sg%d" % i, [128, 512], BF16) for i in range(2)]
                r_sg = [Res("sg0"), Res("sg1")]
                xr = [sb(st, "xr%d" % i, [128, D], F32) for i in range(2)]
                r_xr = [Res("xr0"), Res("xr1")]
                junk = sb(st, "junk", [128, D], BF16)
                r_junk = Res("junk")
                nxr = 0
                for i in range(NTL):
                    ncx.tile(i, src_res)
                    for f in range(NF):
                        sl = f % 2
                        cg = f * 128
                        cu = DFF + f * 128
                        for k in range(KD):
                            mm(pA[sl][:, :], wgu[:, k, cg:cg + 128], ncx.hT[:, k, :], k == 0, k == KD - 1,
                               ncx.r_hT + [wgu_r[cg // gsp]], [r_pA[sl]])
                        for k in range(KD):
                            mm(pB[sl][:, :], wgu[:, k, cu:cu + 128], ncx.hT[:, k, :], k == 0, k == KD - 1,
                               ncx.r_hT + [wgu_r[cu // gsp]], [r_pB[sl]])
                        act(sg[sl][:, :], pA[sl][:, :], AF.Silu, [r_pA[sl]], [r_sg[sl]])
                        tt("dve", actT[:, f, :], sg[sl][:, :], pB[sl][:, :], ALU.mult,
                           [r_sg[sl], r_pB[sl]], [r_actT[f]])
                    for s in range(4):
                        blk = 4 * i + s
                        xs_ = nxr % 2
                        nxr += 1
                        rd = [src_res[blk]] if src_res is not None else []
                        P.dma("sp", [(xr[xs_][:, :], xsrc[blk * 128:(blk + 1) * 128, :])], reads=rd,
                              writes=[r_xr[xs_]], semres=r_xr[xs_])
                        for half in range(2):
                            for f in range(NF):
                                mm(pD[half][:, :], actT[:, f, s * 128:(s + 1) * 128],
                                   wdn[:, f, half * 512:(half + 1) * 512], f == 0, f == NF - 1,
                                   [r_actT[f], wdn_r[(half * 512) // dsp]], [r_pD[half]])
                            stt(xr[xs_][:, half * 512:(half + 1) * 512], pD[half][:, :], 0.5,
                                xr[xs_][:, half * 512:(half + 1) * 512], ALU.mult, ALU.add,
                                [r_pD[half], r_xr[xs_]], [r_xr[xs_]])
                        stt(junk[:, :], xr[xs_][:, :], 1.0, xr[xs_][:, :], ALU.mult, ALU.mult,
                            [r_xr[xs_]], [r_junk, r_ss], accum=ss_all[:, blk:blk + 1])
                        P.dma("sp", [(xs_d[blk * 128:(blk + 1) * 128, :], xr[xs_][:, :])], reads=[r_xr[xs_]],
                              writes=[r_xs[blk]], semres=r_xr[xs_])
                P.end_phase()

        def attn_phases(l):
            with ExitStack() as ast:
                KT = sb(ast, "KT", [128, 4, T], BF16)
                VA = sb(ast, "VA", [128, NB, NHEAD, 65], BF16)
                kiT = sb(ast, "kiT", [128, T], BF16)
                wi_all = sb(ast, "wi_all", [128, NB, 8], F32)
                r_KT = [Res("KT%d" % i) for i in range(NTL)]
                r_VA = [Res("VA%d" % b) for b in range(NB)]
                r_kiT = [Res("kiT%d" % i) for i in range(NTL)]
                r_wi = [Res("wi%d" % b) for b in range(NB)]
                sgn_all = sb(ast, "sgn_all", [128, NB, 8], F32)
                absw_all = sb(ast, "absw_all", [128, NB, 8], F32)
                r_ones = Res("ones")
                with ExitStack() as st:
                    compute_rstd()
                    ncx = NormCtx(st, l * 3 + 1, xs_d)
                    P.op("pool", lambda h: h.memset(VA[:, :, :, 64:65], 1.0), [], [r_ones])
                    wA, wA_r, asp = load_w(st, "wA", wA_d[l], KD, WA_COLS, 512)
                    cs = sb(st, "cs", [128, 512], F32)
                    sn = sb(st, "sn", [128, 512], F32)
                    r_cs, r_sn = Res("cs"), Res("sn")
                    t1 = [sb(st, "t1_%d" % i, [128, 512], F32) for i in range(2)]
                    t2 = [sb(st, "t2_%d" % i, [128, 512], F32) for i in range(2)]
                    r_t1 = [Res("t1a"), Res("t1b")]
                    r_t2 = [Res("t2a"), Res("t2b")]
                    QTt = sb(st, "QTt", [128, 4, 512], BF16)
                    qiTt = sb(st, "qiTt", [128, 4, 512], BF16)
                    r_QTt, r_qiTt = Res("QTt"), Res("qiTt")
                    nrope = 0
                    for i in range(NTL):
                        ncx.tile(i, r_xs)
                        P.dma("sp", [(cs[:, :], cos_d[:, i * 512:(i + 1) * 512])], writes=[r_cs], semres=r_cs)
                        P.dma("sp", [(sn[:, :], sin_d[:, i * 512:(i + 1) * 512])], writes=[r_sn], semres=r_sn)

                        def rope(ca, cb, dest, r_dest):
                            nonlocal nrope
                            sl = nrope % 2
                            nrope += 1
                            for k in range(KD):
                                mm(pA[sl][:, :], wA[:, k, ca:ca + 128], ncx.hT[:, k, :], k == 0, k == KD - 1,
                                   ncx.r_hT + [wA_r[ca // asp]], [r_pA[sl]])
                            for k in range(KD):
                                mm(pB[sl][:, :], wA[:, k, cb:cb + 128], ncx.hT[:, k, :], k == 0, k == KD - 1,
                                   ncx.r_hT + [wA_r[cb // asp]], [r_pB[sl]])
                            tt("dve", t1[sl][:, :], pA[sl][:, :], cs[:, :], ALU.mult, [r_pA[sl], r_cs], [r_t1[sl]])
                            tt("dve", t2[sl][:, :], pB[sl][:, :], sn[:, :], ALU.mult, [r_pB[sl], r_sn], [r_t2[sl]])
                            tt("pool", dest, t1[sl][:, :], t2[sl][:, :], ALU.add, [r_t1[sl], r_t2[sl]], [r_dest])
                        for c in range(4):
                            rope(c * 128, 512 + c * 128, QTt[:, c, :], r_QTt)
                        P.dma("sp", [(qT_d[:, :, i * 512:(i + 1) * 512], QTt[:, :, :])], reads=[r_QTt],
                              writes=[r_qTd[i]], semres=r_QTt)
                        for c in range(4):
                            rope(1024 + c * 128, 1536 + c * 128, KT[:, c, i * 512:(i + 1) * 512], r_KT[i])
                        for c in range(4):
                            rope(2048 + c * 128, 2560 + c * 128, qiTt[:, c, :], r_qiTt)
                        P.dma("sp", [(qiT_d[:, :, i * 512:(i + 1) * 512], qiTt[:, :, :])], reads=[r_qiTt],
                              writes=[r_qiTd[i]], semres=r_qiTt)
                        rope(3072, 3200, kiT[:, i * 512:(i + 1) * 512], r_kiT[i])
                        for s in range(4):
                            blk = 4 * i + s
                            hf = s % 2
                            for k in range(KD):
                                mm(pD[hf][:, :], ncx.hT[:, k, s * 128:(s + 1) * 128], wA[:, k, 3328:3840],
                                   k == 0, k == KD - 1, [ncx.r_hT[s], wA_r[3328 // asp], wA_r[3839 // asp]], [r_pD[hf]])
                            act(VA[:, blk, :, 0:64], pD[hf][:, :].rearrange("p (h d) -> p h d", h=NHEAD), AF.Copy,
                                [r_pD[hf], r_ones], [r_VA[blk]])
                            for k in range(KD):
                                mm(pX[:, 0:8], ncx.hT[:, k, s * 128:(s + 1) * 128], wA[:, k, 3840:3848],
                                   k == 0, k == KD - 1, [ncx.r_hT[s], wA_r[3840 // asp]], [r_pX])
                            ts("dve", wi_all[:, blk, :], pX[:, 0:8], IDX_SCALE, ALU.mult, [r_pX], [r_wi[blk]])
                            act(sgn_all[:, blk, :], wi_all[:, blk, :], AF.Sign, [r_wi[blk]], [r_wi[blk]])
                            tt("dve", absw_all[:, blk, :], wi_all[:, blk, :], sgn_all[:, blk, :], ALU.mult, [r_wi[blk]], [r_wi[blk]])
                    P.end_phase()
                if stop_after == "MA1":
                    return
                with ExitStack() as st:
                    QTt = [sb(st, "QTt%d" % i, [128, 4, 512], BF16) for i in range(2)]
                    qiTt = [sb(st, "qiTt%d" % i, [128, 4, 512], BF16) for i in range(2)]
                    r_QTt = [Res("QTt0"), Res("QTt1")]
                    r_qiTt = [Res("qiTt0"), Res("qiTt1")]
                    S = [sb(st, "S%d" % i, [128, T], F32) for i in range(2)]
                    r_S = [[Res("S%d_%d" % (i, c)) for c in range(NTL)] for i in range(2)]
                    rb = [sb(st, "rb%d" % i, [128, 512], BF16) for i in range(5)]
                    r_rb = [Res("rb%d" % i) for i in range(5)]
                    Dh = [sb(st, "Dh%d" % i, [128, 8, 128], BF16) for i in range(2)]
                    r_Dh = [Res("Dh0"), Res("Dh1")]
                    dmask = sb(st, "dmask", [128, 128], F32)
                    r_dmask = Res("dmask")
                    P.dma("sp", [(dmask[:, :], dmask_d)], writes=[r_dmask], semres=r_dmask)
                    junkb = sb(st, "junkb", [128, T], BF16)
                    r_junkb = Res("junkb")
                    sv = sb(st, "sv", [128, 8], F32)
                    r_sv = Res("sv")
                    pwt = sb(st, "pwt", [128, 48], F32)
                    r_pwt = Res("pwt")
                    P.dma("sp", [(pwt[:, :], pwt_d)], writes=[r_pwt], semres=r_pwt)
                    qtab = sb(st, "qtab", [128, 48], F32)
                    r_qtab = Res("qtab")
                    MB = sb(st, "MB", [128, T], BF16)
                    r_MB = Res("MB")
                    MBT = [sb(st, "MBT%d" % i, [128, NB, 128], BF16) for i in range(2)]
                    r_MBT = [Res("MBT0"), Res("MBT1")]
                    PT = [sb(st, "PT%d" % i, [128, 512], BF16) for i in range(5)]
                    r_PT = [Res("PT%d" % i) for i in range(5)]
                    PTm = [sb(st, "PTm%d" % i, [128, 512], BF16) for i in range(5)]
                    r_PTm = [Res("PTm%d" % i) for i in range(5)]
                    junka = sb(st, "junka", [128, T], BF16)
                    r_junka = Res("junka")
                    obr = [sb(st, "obr%d" % i, [128, NHEAD, 65], F32) for i in range(2)]
                    r_obr = [Res("obr0"), Res("obr1")]
                    ob = sb(st, "ob", [128, 512], BF16)
                    r_ob = Res("ob")
                    rc = sb(st, "rc", [128, 8], F32)
                    r_rc = Res("rc")
                    oTq = [sb(st, "oTq%d" % i, [128, 4, 128], BF16) for i in range(2)]
                    r_oTq = [Res("oTq0"), Res("oTq1")]
                    cnt_ = {"rl": 0, "pt": 0}

                    def stage_A1(qb):
                        i = qb // 4
                        tq = qb % 4
                        ts_ = i % 2
                        if tq == 0:
                            P.dma("sp", [(QTt[ts_][:, :, :], qT_d[:, :, i * 512:(i + 1) * 512])], reads=[r_qTd[i]],
                                  writes=[r_QTt[ts_]], semres=r_QTt[ts_])
                            P.dma("sp", [(qiTt[ts_][:, :, :], qiT_d[:, :, i * 512:(i + 1) * 512])], reads=[r_qiTd[i]],
                                  writes=[r_qiTt[ts_]], semres=r_qiTt[ts_])
                        L = (qb + 1) * 128
                        nch = (L + 511) // 512
                        ss_ = qb % 2
                        Sq = S[ss_]
                        ds_ = qb % 2
                        for h in range(8):
                            ts("dve", Dh[ds_][:, h, :], ident[:, :], sgn_all[:, qb, h:h + 1], ALU.mult,
                               [r_ident, r_wi[qb]], [r_Dh[ds_]])
                        yield
                        pend = []

                        def emit_sum(item):
                            c, N, h, rs_ = item
                            mm(pX[:, 0:N], Dh[ds_][:, h, :], rb[rs_][:, 0:N], h == 0, h == 7,
                               [r_Dh[ds_], r_rb[rs_]], [r_pX], signal=True)
                            if h == 7:
                                c0 = c * 512
                                if c == nch - 1:
                                    if N > 128:
                                        cp("dve", Sq[:, c0:c0 + N - 128], pX[:, 0:N - 128], [r_pX], [r_S[ss_][c]])
                                    tt("dve", Sq[:, L - 128:L], pX[:, N - 128:N], dmask[:, :], ALU.add,
                                       [r_pX, r_dmask], [r_S[ss_][c]])
                                else:
                                    cp("dve", Sq[:, c0:c0 + N], pX[:, 0:N], [r_pX], [r_S[ss_][c]])
                        for c in range(nch):
                            N = min(512, L - 512 * c)
                            for h in range(8):
                                ph = 64 * (h % 2)
                                pb_ = h % 2
                                mm(pB[pb_][:, 0:N], qiTt[ts_][ph:ph + 64, h // 2, tq * 128:(tq + 1) * 128],
                                   kiT[ph:ph + 64, c * 512:c * 512 + N], True, True,
                                   [r_qiTt[ts_], r_kiT[c]], [r_pB[pb_]])
                                rs_ = cnt_["rl"] % 5
                                cnt_["rl"] += 1
                                act(rb[rs_][:, 0:N], pB[pb_][:, 0:N], AF.Relu, [r_pB[pb_], r_wi[qb]], [r_rb[rs_]],
                                    scale=absw_all[:, qb, h:h + 1])
                                pend.append((c, N, h, rs_))
                                while len(pend) > 2:
                                    emit_sum(pend.pop(0))
                                yield
                        while pend:
                            emit_sum(pend.pop(0))
                            yield

                    def stage_A2(qb):
                        L = (qb + 1) * 128
                        nch = (L + 511) // 512
                        ss_ = qb % 2
                        Sq = S[ss_]
                        rS = r_S[ss_][:nch]
                        if L > topk:
                            P.op("dve", lambda h: h.tensor_reduce(out=sv[:, 0:1], in_=Sq[:, 0:L - 128], axis=AX.X, op=ALU.max,
                                                                  apply_absolute_value=True), rS, [r_sv])
                            yield
                            ts("dve", sv[:, 2:3], sv[:, 0:1], 2.0, ALU.mult, [r_sv], [r_sv])
                            yield
                            ts("dve", qtab[:, :], pwt[:, :], sv[:, 2:3], ALU.mult, [r_pwt, r_sv], [r_qtab])
                            yield
                            ts("dve", sv[:, 3:4], sv[:, 0:1], 0.0, ALU.mult, [r_sv], [r_sv])
                            yield
                            for n in range(NIT):
                                if n % 5 in (1, 3):
                                    act(junka[:, 0:L], Sq[:, 0:L], AF.Sign, rS + [r_sv], [r_junka, r_sv], scale=-1.0,
                                        bias=sv[:, 3:4], accum=sv[:, 4:5])
                                    yield
                                    ts("dve", sv[:, 5:6], sv[:, 4:5], float(L - 2 * topk) + 0.5, ALU.is_le, [r_sv, r_qtab], [r_sv],
                                       s2=qtab[:, 24 + n:25 + n], op1=ALU.mult)
                                    yield
                                else:
                                    ts("dve", junkb[:, 0:L], Sq[:, 0:L], sv[:, 3:4], ALU.is_ge, rS + [r_sv], [r_junkb, r_sv],
                                       s2=0.0, op1=ALU.add, accum=sv[:, 4:5])
                                    yield
                                    ts("dve", sv[:, 5:6], sv[:, 4:5], float(topk) - 0.5, ALU.is_ge, [r_sv, r_qtab], [r_sv],
                                       s2=qtab[:, 24 + n:25 + n], op1=ALU.mult)
                                    yield
                                stt(sv[:, 3:4], sv[:, 5:6], qtab[:, n:n + 1], sv[:, 3:4], ALU.subtract, ALU.add,
                                    [r_sv, r_qtab], [r_sv])
                                yield
                            tt("dve", sv[:, 1:2], sv[:, 3:4], qtab[:, 24 + NIT:25 + NIT], ALU.subtract, [r_sv, r_qtab], [r_sv])
                            yield
                        else:
                            P.op("dve", lambda h: h.memset(sv[:, 1:2], -1e29), [], [r_sv])
                            yield

                    def stage_A3(qb):
                        L = (qb + 1) * 128
                        nch = (L + 511) // 512
                        ss_ = qb % 2
                        Sq = S[ss_]
                        rS = r_S[ss_][:nch]
                        ts("dve", MB[:, 0:L], Sq[:, 0:L], sv[:, 1:2], ALU.is_ge, rS + [r_sv], [r_MB])
                        ms = qb % 2
                        for j0 in range(0, qb + 1, 8):
                            nj = min(8, qb + 1 - j0)
                            for jj in range(nj):
                                j = j0 + jj
                                tr(pT[:, jj * 128:(jj + 1) * 128], MB[:, j * 128:(j + 1) * 128], [r_MB], [r_pT],
                                   signal=(jj == nj - 1))
                            cp("dve", MBT[ms][:, j0:j0 + nj, :], pT[:, 0:nj * 128].rearrange("p (j t) -> p j t", j=nj),
                               [r_pT], [r_MBT[ms]])

                    def stage_B(qb):
                        i = qb // 4
                        tq = qb % 4
                        ts_ = i % 2
                        ms = qb % 2
                        os_ = qb % 2
                        pend = []

                        def emit_pv(item):
                            kind = item[0]
                            if kind == "pv":
                                _, h, jg, nj, pts = item
                                po = pD[h % 2]
                                for jj in range(nj):
                                    j = jg + jj
                                    mm(po[:, 0:65], PTm[pts][:, jj * 128:(jj + 1) * 128], VA[:, j, h, :],
                                       j == 0, j == qb, [r_PTm[pts], r_VA[j], r_ones], [r_pD[h % 2]],
                                       signal=(jj == nj - 1))
                            else:
                                _, h = item
                                act(obr[os_][:, h, :], pD[h % 2][:, 0:65], AF.Copy, [r_pD[h % 2]], [r_obr[os_]])
                        for h in range(NHEAD):
                            ph = 64 * (h % 2)
                            for jg in range(0, qb + 1, 4):
                                nj = min(4, qb + 1 - jg)
                                pa_ = cnt_["pt"] % 2
                                for jj in range(nj):
                                    j = jg + jj
                                    mm(pA[pa_][:, jj * 128:(jj + 1) * 128], KT[ph:ph + 64, h // 2, j * 128:(j + 1) * 128],
                                       QTt[ts_][ph:ph + 64, h // 2, tq * 128:(tq + 1) * 128], True, True,
                                       [r_KT[j // 4], r_QTt[ts_]], [r_pA[pa_]], signal=(jj == nj - 1))
                                pts = cnt_["pt"] % 5
                                cnt_["pt"] += 1
                                act(PT[pts][:, 0:nj * 128], pA[pa_][:, 0:nj * 128], AF.Exp, [r_pA[pa_]], [r_PT[pts]],
                                    scale=ATT_SCALE)
                                tt("pool", PTm[pts][:, 0:nj * 128], PT[pts][:, 0:nj * 128],
                                   MBT[ms][:, jg:jg + nj, :].rearrange("p j t -> p (j t)"), ALU.mult,
                                   [r_PT[pts], r_MBT[ms]], [r_PTm[pts]])
                                pend.append(("pv", h, jg, nj, pts))
                                if jg + 4 > qb:
                                    pend.append(("evac", h))
                                while len([p for p in pend if p[0] == "pv"]) > 2:
                                    emit_pv(pend.pop(0))
                                    while pend and pend[0][0] == "evac":
                                        emit_pv(pend.pop(0))
                                yield
                        while pend:
                            emit_pv(pend.pop(0))
                            yield

                    def stage_C(qb):
                        os_ = qb % 2
                        P.op("dve", lambda h_: h_.reciprocal(out=rc[:, :], in_=obr[os_][:, :, 64]), [r_obr[os_]], [r_rc])
                        for h in range(NHEAD):
                            ts("dve", ob[:, h * 64:(h + 1) * 64], obr[os_][:, h, 0:64], rc[:, h:h + 1], ALU.mult,
                               [r_obr[os_], r_rc], [r_ob])
                        for c in range(4):
                            tr(pT[:, c * 128:(c + 1) * 128], ob[:, c * 128:(c + 1) * 128], [r_ob], [r_pT],
                               signal=(c == 3))
                        cp("dve", oTq[os_][:, :, :], pT[:, 0:512].rearrange("p (c t) -> p c t", c=4), [r_pT], [r_oTq[os_]])
                        P.dma("sp", [(oT_d[:, :, qb * 128:(qb + 1) * 128], oTq[os_][:, :, :])], reads=[r_oTq[os_]],
                              writes=[r_oTd[qb]], semres=r_oTq[os_])

                    def run_all(gens):
                        gens = [g for g in gens if g is not None]
                        while gens:
                            for g in list(gens):
                                try:
                                    next(g)
                                except StopIteration:
                                    gens.remove(g)

                    run_all([stage_A1(0)])
                    for qb in range(NB):
                        run_all([stage_A2(qb),
                                 stage_A1(qb + 1) if qb + 1 < NB else None,
                                 stage_B(qb - 1) if qb >= 1 else None])
                        stage_A3(qb)
                        if qb >= 1:
                            stage_C(qb - 1)
                    run_all([stage_B(NB - 1)])
                    stage_C(NB - 1)
                    P.end_phase()

        def mb_phase(l):
            with ExitStack() as st:
                ncx = NormCtx(st, l * 3 + 1, xs_d)
                wB, wB_r, bsp = load_w(st, "wB", wB_d[l], KD, WB_COLS, 512)
                wpa, wpa_r, _ = load_w(st, "wpa", wpa_d[l], 4, D, 1024)
                wpb, wpb_r, _ = load_w(st, "wpb", wpb_d[l], 4, D, 1024)
                wout, wout_r, _ = load_w(st, "wout", wout_d[l], KD, D, 1024)
                bgT = sb(st, "bgT", [128, 16], F32)
                lng = sb(st, "lng", [128, 512], F32)
                lnb = sb(st, "lnb", [128, 512], F32)
                bsb = sb(st, "bsb", [128, 512], F32)
                wsf = sb(st, "wsf", [128, 512], F32)
                trl = sb(st, "trl", [128, 512], F32)
                wsT = sb(st, "wsT", [128, 512], BF16)
                r_c = Res("mbconst")
                r_wsf, r_trl, r_wsT = Res("wsf"), Res("trl"), Res("wsT")
                P.dma("sp", [(bgT[:, :], bgT_d[l]),
                             (lng[:, :], lng_d[l:l + 1, :].broadcast_to([128, 512])),
                             (lnb[:, :], lnb_d[l:l + 1, :].broadcast_to([128, 512])),
                             (bsb[:, :], bsb_d[l:l + 1, :].broadcast_to([128, 512]))], writes=[r_c], semres=r_c)
                P.dma("sp", [(wsf[:, :], wsT_d[l])], writes=[r_wsf], semres=r_wsf)
                P.dma("sp", [(trl[:, :], tril_d)], writes=[r_trl], semres=r_trl)
                tt("dve", wsT[:, :], wsf[:, :], trl[:, :], ALU.mult, [r_wsf, r_trl], [r_wsT])
                guT = sb(st, "guT", [128, 4, 512], BF16)
                r_guT = [Res("guT%d" % c) for c in range(4)]
                gT = sb(st, "gT", [128, 16, 512], BF16)
                r_gT = [Res("gT%d" % c) for c in range(16)]
                gv = [sb(st, "gv%d" % i, [128, 512], F32) for i in range(4)]
                r_gv = [Res("gv%d" % i) for i in range(4)]
                vb = [sb(st, "vb%d" % i, [128, 512], BF16) for i in range(4)]
                r_vb = [Res("vb%d" % i) for i in range(4)]
                st6 = sb(st, "st6", [128, 4, 6], F32)
                mv = sb(st, "mv", [128, 4, 2], F32)
                lr = sb(st, "lr", [128, 4], F32)
                r_st6 = [Res("st6_%d" % i) for i in range(4)]
                r_mv = [Res("mv%d" % i) for i in range(4)]
                r_lr = [Res("lr%d" % i) for i in range(4)]
                negh = sb(st, "negh", [128, 2], F32)
                r_negh = Res("negh")
                P.op("pool", lambda h: h.memset(negh[:, :], -0.5), [], [r_negh])
                yapT = sb(st, "yapT", [128, 4, 512], BF16)
                r_yap = [Res("yap%d" % s) for s in range(4)]
                tmpf = [sb(st, "tmpf%d" % i, [128, 512], F32) for i in range(2)]
                r_tmpf = [Res("tmpf0"), Res("tmpf1")]
                tmpg = [sb(st, "tmpg%d" % i, [128, 512], F32) for i in range(2)]
                r_tmpg = [Res("tmpg0"), Res("tmpg1")]
                oTt = [sb(st, "oTt%d" % i, [128, 4, 512], BF16) for i in range(2)]
                r_oTt = [Res("oTt0"), Res("oTt1")]
                zT = sb(st, "zT", [128, 8, 512], BF16)
                r_zT = [Res("zT%d" % m) for m in range(8)]
                xr = [sb(st, "xr%d" % i, [128, D], F32) for i in range(2)]
                r_xr = [Res("xr0"), Res("xr1")]
                junk = sb(st, "junk", [128, D], BF16)
                r_junk = Res("junk")
                nxr = 0
                ngv = 0
                ntm = 0
                for i in range(NTL):
                    ncx.tile(i, r_xs)
                    os_ = i % 2
                    P.dma("sp", [(oTt[os_][:, :, :], oT_d[:, :, i * 512:(i + 1) * 512])],
                          reads=r_oTd[4 * i:4 * i + 4], writes=[r_oTt[os_]], semres=r_oTt[os_])
                    for c in range(4):
                        sl = c % 2
                        for k in range(KD):
                            mm(pA[sl][:, :], wB[:, k, c * 128:(c + 1) * 128], ncx.hT[:, k, :], k == 0, k == KD - 1,
                               ncx.r_hT + [wB_r[(c * 128) // bsp]], [r_pA[sl]])
                        act(guT[:, c, :], pA[sl][:, :], AF.Gelu_apprx_tanh, [r_pA[sl]], [r_guT[c]])
                    for s in range(4):
                        hf = s % 2
                        for k in range(KD):
                            mm(pD[hf][:, :], ncx.hT[:, k, s * 128:(s + 1) * 128], wB[:, k, 2560:3072], k == 0, k == KD - 1,
                               [ncx.r_hT[s], wB_r[2560 // bsp]], [r_pD[hf]])
                        g_ = s
                        ngv += 1
                        act(gv[g_][:, :], pD[hf][:, :], AF.Gelu_apprx_tanh, [r_pD[hf]], [r_gv[g_]])
                        P.op("dve", lambda h, g_=g_: h.bn_stats(out=st6[:, g_, :], in_=gv[g_][:, :]), [r_gv[g_]], [r_st6[g_]])
                        P.op("dve", lambda h, g_=g_: h.bn_aggr(out=mv[:, g_, :], in_=st6[:, g_, :]), [r_st6[g_]], [r_mv[g_]])
                        ts("dve", lr[:, g_:g_ + 1], mv[:, g_, 1:2], EPS, ALU.add, [r_mv[g_]], [r_lr[g_]])
                        tt("pool", lr[:, g_:g_ + 1], lr[:, g_:g_ + 1], negh[:, 0:1], ALU.pow, [r_lr[g_], r_negh], [r_lr[g_]])
                        ts("dve", gv[g_][:, :], gv[g_][:, :], mv[:, g_, 0:1], ALU.subtract, [r_gv[g_], r_mv[g_], r_lr[g_]],
                           [r_gv[g_]], s2=lr[:, g_:g_ + 1], op1=ALU.mult)
                        tt("dve", gv[g_][:, :], gv[g_][:, :], lng[:, :], ALU.mult, [r_gv[g_], r_c], [r_gv[g_]])
                        tt("dve", vb[g_][:, :], gv[g_][:, :], lnb[:, :], ALU.add, [r_gv[g_], r_c], [r_vb[g_]])
                    for c in range(16):
                        sl = c % 2
                        c0 = 512 + c * 128
                        for k in range(KD):
                            mm(pB[sl][:, :], wB[:, k, c0:c0 + 128], ncx.hT[:, k, :], k == 0, k == KD - 1,
                               ncx.r_hT + [wB_r[c0 // bsp]], [r_pB[sl]])
                        act(gT[:, c, :], pB[sl][:, :], AF.Sigmoid, [r_pB[sl], r_c], [r_gT[c]], bias=bgT[:, c:c + 1])
                    for s in range(4):
                        g_ = s
                        for g in range(4):
                            mm(pX[:, g * 128:(g + 1) * 128], vb[g_][:, g * 128:(g + 1) * 128], wsT[:, g * 128:(g + 1) * 128],
                               True, True, [r_vb[g_], r_wsT], [r_pX])
                        t_ = ntm % 2
                        ntm += 1
                        tt("dve", tmpf[t_][:, :], pX[:, :], bsb[:, :], ALU.add, [r_pX, r_c], [r_tmpf[t_]])
                        tt("dve", yapT[:, :, s * 128:(s + 1) * 128], tmpf[t_][:, :].rearrange("p (g t) -> p g t", g=4),
                           guT[:, :, s * 128:(s + 1) * 128], ALU.mult, [r_tmpf[t_]] + r_guT, [r_yap[s]])
                    for m in range(8):
                        sl = m % 2
                        for g in range(4):
                            mm(pA[sl][:, :], wpa[:, g, m * 128:(m + 1) * 128], yapT[:, g, :], g == 0, g == 3,
                               r_yap + wpa_r, [r_pA[sl]])
                        for c in range(4):
                            mm(pB[sl][:, :], wpb[:, c, m * 128:(m + 1) * 128], oTt[os_][:, c, :], c == 0, c == 3,
                               [r_oTt[os_]] + wpb_r, [r_pB[sl]])
                        tt("dve", tmpf[sl][:, :], pA[sl][:, :], gT[:, m, :], ALU.mult, [r_pA[sl], r_gT[m]], [r_tmpf[sl]])
                        tt("dve", tmpg[sl][:, :], pB[sl][:, :], gT[:, 8 + m, :], ALU.mult, [r_pB[sl], r_gT[8 + m]], [r_tmpg[sl]])
                        tt("pool", zT[:, m, :], tmpf[sl][:, :], tmpg[sl][:, :], ALU.add, [r_tmpf[sl], r_tmpg[sl]], [r_zT[m]])
                    for s in range(4):
                        blk = 4 * i + s
                        xs_ = nxr % 2
                        nxr += 1
                        P.dma("sp", [(xr[xs_][:, :], xs_d[blk * 128:(blk + 1) * 128, :])], reads=[r_xs[blk]],
                              writes=[r_xr[xs_]], semres=r_xr[xs_])
                        for half in range(2):
                            for j in range(8):
                                mm(pD[half][:, :], zT[:, j, s * 128:(s + 1) * 128], wout[:, j, half * 512:(half + 1) * 512],
                                   j == 0, j == 7, [r_zT[j]] + wout_r, [r_pD[half]])
                            tt("dve", xr[xs_][:, half * 512:(half + 1) * 512], pD[half][:, :],
                               xr[xs_][:, half * 512:(half + 1) * 512], ALU.add, [r_pD[half], r_xr[xs_]], [r_xr[xs_]])
                        stt(junk[:, :], xr[xs_][:, :], 1.0, xr[xs_][:, :], ALU.mult, ALU.mult,
                            [r_xr[xs_]], [r_junk, r_ss], accum=ss_all[:, blk:blk + 1])
                        P.dma("sp", [(xs_d[blk * 128:(blk + 1) * 128, :], xr[xs_][:, :])], reads=[r_xr[xs_]],
                              writes=[r_xs[blk]], semres=r_xr[xs_])
                P.end_phase()

        def final_phase(src, src_res):
            with ExitStack() as st:
                compute_rstd()
                gb = sb(st, "fgb", [128, D], F32)
                r_gb = Res("fgb")
                P.dma("sp", [(gb[:, :], nrm_d[DEPTH * 3:DEPTH * 3 + 1, :].broadcast_to([128, D]))], writes=[r_gb], semres=r_gb)
                xr = [sb(st, "fxr%d" % i, [128, D], F32) for i in range(2)]
                r_xr = [Res("fxr0"), Res("fxr1")]
                toks = []
                for blk in range(NB):
                    sl = blk % 2
                    rd = [src_res[blk]] if src_res is not None else []
                    P.dma("sp", [(xr[sl][:, :], src[blk * 128:(blk + 1) * 128, :])], reads=rd, writes=[r_xr[sl]], semres=r_xr[sl])
                    stt(xr[sl][:, :], xr[sl][:, :], rstd_all[:, blk:blk + 1], gb[:, :], ALU.mult, ALU.mult,
                        [r_xr[sl], r_rstd, r_gb], [r_xr[sl]])
                    P.dma("sp", [(out_d[blk * 128:(blk + 1) * 128, :], xr[sl][:, :])], reads=[r_xr[sl]], writes=[r_out], semres=r_xr[sl])
                P.end_phase()

        done = False
        src, src_res = x_d, None
        for l in range(DEPTH):
            ffn_phase(l, 0, src, src_res, first=(l == 0))
            src, src_res = xs_d, r_xs
            if stop_after == "F1":
                done = True
                break
            attn_phases(l)
            if stop_after in ("MA1", "MA2"):
                done = True
                break
            mb_phase(l)
            if stop_after == "MB":
                done = True
                break
            ffn_phase(l, 1, xs_d, r_xs)
        if stop_after is None or stop_after in ("F1", "MB"):
            final_phase(src, src_res)
        else:
            final_phase(x_d, None)
        build.info = dict(ninst=P.ninst, nsem=P.nsem, nblocks=P.nblocks)
    return nc


def _swap_cols(n_heads, hd=64):
    idx = []
    for h in range(n_heads):
        idx += list(range(h * hd + hd // 2, (h + 1) * hd)) + list(range(h * hd, h * hd + hd // 2))
    return np.array(idx)


def prep_shared(inp, T, DEPTH, DFF):
    f32 = np.float32
    w_in = np.asarray(inp["w_in"], f32)
    o = {}
    u0, va0, q0, k0, v0, qi0, ki0, wi0, ga0, gb0 = 0, 512, 1024, 1536, 2048, 2560, 3072, 3136, 3144, 4168
    sw8 = _swap_cols(8)
    sw1 = _swap_cols(1)
    q = w_in[:, :, q0:q0 + 512]
    k = w_in[:, :, k0:k0 + 512]
    qi = w_in[:, :, qi0:qi0 + 512]
    ki = w_in[:, :, ki0:ki0 + 64]
    ki_sw = ki[:, :, sw1]
    wA = np.concatenate([q, q[:, :, sw8], k, k[:, :, sw8], qi, qi[:, :, sw8], ki, ki, ki_sw, ki_sw,
                         w_in[:, :, v0:v0 + 512], w_in[:, :, wi0:wi0 + 8]], axis=2)
    assert wA.shape[2] == WA_COLS
    wB = np.concatenate([w_in[:, :, u0:u0 + 512], w_in[:, :, ga0:ga0 + 1024], w_in[:, :, gb0:gb0 + 1024],
                         w_in[:, :, va0:va0 + 512]], axis=2)
    assert wB.shape[2] == WB_COLS
    o["wA"] = np.ascontiguousarray(wA)
    o["wB"] = np.ascontiguousarray(wB)
    nrm = np.stack([np.asarray(inp["ffn1_norm"], f32), np.asarray(inp["mix_norm"], f32),
                    np.asarray(inp["ffn2_norm"], f32)], axis=1).reshape(DEPTH * 3, D)
    o["nrm"] = np.ascontiguousarray(np.concatenate([nrm, np.asarray(inp["final_norm"], f32)[None, :]], axis=0))
    o["wgu1"] = np.ascontiguousarray(inp["ffn1_w_gu"], f32)
    o["wgu2"] = np.ascontiguousarray(inp["ffn2_w_gu"], f32)
    o["wdn1"] = np.ascontiguousarray(inp["ffn1_w_down"], f32)
    o["wdn2"] = np.ascontiguousarray(inp["ffn2_w_down"], f32)
    bg = np.asarray(inp["b_gate"], f32)
    o["bgT"] = np.ascontiguousarray(bg.reshape(DEPTH, 16, 128).transpose(0, 2, 1))
    o["lng"] = np.ascontiguousarray(inp["gmlp_ln_g"], f32)
    o["lnb"] = np.ascontiguousarray(inp["gmlp_ln_b"], f32)
    ws = np.asarray(inp["gmlp_w_s"], f32)
    o["wsT"] = np.ascontiguousarray(ws.transpose(0, 3, 1, 2).reshape(DEPTH, 128, 512))
    o["bsb"] = np.ascontiguousarray(np.asarray(inp["gmlp_b_s"], f32).reshape(DEPTH, 512))
    o["wpa"] = np.ascontiguousarray(inp["w_pa"], f32)
    o["wpb"] = np.ascontiguousarray(inp["w_pb"], f32)
    o["wout"] = np.ascontiguousarray(inp["w_out"], f32)
    o["ident"] = np.eye(128, dtype=f32).astype(ml_dtypes.bfloat16)
    inv = (np.float32(10000.0) ** (-np.arange(0, 64, 2, dtype=f32) / np.float32(64))).astype(f32)
    ang = np.arange(T, dtype=f32)[:, None] * inv[None, :]
    cos = np.cos(ang).astype(f32).T
    sin = np.sin(ang).astype(f32).T
    cos64 = np.concatenate([cos, cos], axis=0)
    sin64 = np.concatenate([-sin, sin], axis=0)
    o["cosT"] = np.ascontiguousarray(np.concatenate([cos64, cos64], axis=0))
    o["sinT"] = np.ascontiguousarray(np.concatenate([sin64, sin64], axis=0))
    tq = np.arange(128)
    o["dmask"] = np.where(tq[None, :] <= tq[:, None], 0.0, -1e30).astype(f32)
    tr1 = (tq[:, None] <= tq[None, :]).astype(f32)
    o["tril"] = np.ascontiguousarray(np.tile(tr1, (1, 4)))
    pw = np.concatenate([2.0 ** -(np.arange(24) + 2.0), 2.0 ** -(np.arange(24) + 1.0)]).astype(f32)
    o["pwt"] = np.ascontiguousarray(np.tile(pw[None, :], (128, 1)))
    return o


_T, _DEPTH, _DFF = 4096, 2, 2816


def kernel(**inputs):
    x = np.asarray(inputs["x"], np.float32)
    B = x.shape[0]
    shared = prep_shared(inputs, _T, _DEPTH, _DFF)
    nc = build(_T, _DEPTH, _DFF)
    in_maps = []
    for b in range(B):
        m = dict(shared)
        m["x"] = np.ascontiguousarray(x[b])
        in_maps.append(m)
    res = run_bass_kernel_spmd(nc, in_maps, core_ids=list(range(B)))
    return np.stack([np.asarray(r["out"], np.float32) for r in res.results], axis=0)
```

```python
import numpy as np
import ml_dtypes
from contextlib import ExitStack
import concourse.bass as bass
import concourse.mybir as mybir
from concourse.bass_utils import run_bass_kernel_spmd

F32 = mybir.dt.float32
BF16 = mybir.dt.bfloat16
U32 = mybir.dt.uint32
AF = mybir.ActivationFunctionType
ALU = mybir.AluOpType
AX = mybir.AxisListType

D = 1024
KD = D // 128
NHEAD = 8
HDIM = 64
TOPK_MAX = 256
EPS = 1e-6
IDX_SCALE = (64 ** -0.5) * (8 ** -0.5)
ATT_SCALE = 64 ** -0.5
NEG = -30000.0
WA_COLS = 3848
WB_COLS = 3072


class Res:
    __slots__ = ("name", "w", "rs")

    def __init__(self, name):
        self.name = name
        self.w = None
        self.rs = []


class Eng:
    def __init__(self, name, sem):
        self.name = name
        self.sem = sem
        self.count = 0
        self.ops = []
        self.known = {}


class Prog:
    def __init__(self, nc, stack):
        self.nc = nc
        self.stack = stack
        self.eng = {}
        self.nsem = 0
        for n in ("pe", "act", "dve", "pool", "sp"):
            self.eng[n] = Eng(n, self.new_sem("sem_" + n))
        self._dsem = {}
        self._dpool = {}
        self._dtoks = []
        self.nblocks = 0
        self.ninst = 0

    def new_sem(self, name):
        s = self.stack.enter_context(self.nc.semaphore(name))
        self.nsem += 1
        return s

    def _collect(self, e, reads, writes, is_dma):
        waits = {}

        def add(tok, same_ok):
            if tok is None:
                return
            sem, val, en = tok
            if en == e.name and same_ok:
                return
            if e.known.get(id(sem), 0) >= val:
                return
            k = id(sem)
            if k not in waits or waits[k][1] < val:
                waits[k] = tok
        pe = (e.name == "pe") and not is_dma
        for r in reads:
            add(r.w, pe)
        for r in writes:
            add(r.w, pe)
            for t in r.rs:
                add(t, pe)
        for t in waits.values():
            e.known[id(t[0])] = max(e.known.get(id(t[0]), 0), t[1])
        return list(waits.values())

    def op(self, en, fn, reads=(), writes=(), signal=True):
        e = self.eng[en]
        waits = self._collect(e, reads, writes, False)
        if signal:
            e.count += 1
            tok = (e.sem, e.count, en)
            e.ops.append((waits, fn, (e.sem, 1)))
        else:
            tok = (e.sem, e.count + 1, en)
            e.ops.append((waits, fn, None))
        for r in reads:
            r.rs.append(tok)
        for r in writes:
            r.w = tok
            r.rs = []
        return tok

    def dma(self, en, pairs, reads=(), writes=(), semres=None):
        e = self.eng[en]
        key = id(semres)
        if key not in self._dsem:
            pool_ = self._dpool.setdefault(en, [])
            if pool_:
                ent = pool_.pop()
                ent[2] = Res("ser")
            else:
                ent = [self.new_sem("dsem%d" % self.nsem), 0, Res("ser"), en]
            self._dsem[key] = ent
        ent = self._dsem[key]
        reads = list(reads)
        writes = list(writes) + [ent[2]]
        waits = self._collect(e, reads, writes, True)
        ent[1] += 16 * len(pairs)
        tok = (ent[0], ent[1], None)
        for i, (o, a) in enumerate(pairs):
            e.ops.append((waits if i == 0 else [],
                          (lambda h, o=o, a=a: h.dma_start(out=o, in_=a)), (ent[0], 16)))
        for r in reads:
            r.rs.append(tok)
        for r in writes:
            r.w = tok
            r.rs = []
        return tok

    def end_phase(self):
        sp = self.eng["sp"]
        for ent in self._dsem.values():
            if ent[1] > 0:
                sp.ops.append(([(ent[0], ent[1], None)], None, None))
        for ent in self._dsem.values():
            self._dpool[ent[3]].append(ent)
        self._dsem = {}
        self.emit()

    def emit(self):
        nc = self.nc
        handles = {"pe": "tensor", "act": "scalar", "dve": "vector", "pool": "gpsimd", "sp": "sync"}
        with nc.Block() as block:
            for en, attr in handles.items():
                e = self.eng[en]

                def body(h, e=e):
                    for waits, fn, inc in e.ops:
                        for (sem, val, _) in waits:
                            h.wait_ge(sem, val)
                        if fn is not None:
                            ins = fn(h)
                            if inc is not None:
                                ins.then_inc(inc[0], inc[1])
                getattr(block, attr)(body)
                self.ninst += len(e.ops)
                e.ops = []
        self.nblocks += 1


def build(T, DEPTH, DFF, NIT=15, stop_after=None):
    NB = T // 128
    NTL = T // 512
    NF = DFF // 128
    topk = min(TOPK_MAX, T // 4)
    nc = bass.Bass("TRN2", target_bir_lowering=False)

    def din(name, shape, dt=F32):
        return nc.dram_tensor(name, list(shape), dt, kind="ExternalInput").ap()

    x_d = din("x", [T, D])
    nrm_d = din("nrm", [DEPTH * 3 + 1, D])
    wgu_d = [din("wgu1", [DEPTH, D, 2 * DFF]), din("wgu2", [DEPTH, D, 2 * DFF])]
    wdn_d = [din("wdn1", [DEPTH, DFF, D]), din("wdn2", [DEPTH, DFF, D])]
    wA_d = din("wA", [DEPTH, D, WA_COLS])
    wB_d = din("wB", [DEPTH, D, WB_COLS])
    bgT_d = din("bgT", [DEPTH, 128, 16])
    lng_d = din("lng", [DEPTH, 512])
    lnb_d = din("lnb", [DEPTH, 512])
    wsT_d = din("wsT", [DEPTH, 128, 512])
    bsb_d = din("bsb", [DEPTH, 512])
    wpa_d = din("wpa", [DEPTH, 512, D])
    wpb_d = din("wpb", [DEPTH, 512, D])
    wout_d = din("wout", [DEPTH, D, D])
    ident_d = din("ident", [128, 128], BF16)
    cos_d = din("cosT", [128, T])
    sin_d = din("sinT", [128, T])
    dmask_d = din("dmask", [128, 128])
    tril_d = din("tril", [128, 512])
    pwt_d = din("pwt", [128, 48])
    out_d = nc.dram_tensor("out", [T, D], F32, kind="ExternalOutput").ap()
    xs_d = nc.dram_tensor("xs", [T, D], F32, kind="Internal").ap()
    qT_d = nc.dram_tensor("qT_s", [128, 4, T], BF16, kind="Internal").ap()
    qiT_d = nc.dram_tensor("qiT_s", [128, 4, T], BF16, kind="Internal").ap()
    oT_d = nc.dram_tensor("oT_s", [128, 4, T], BF16, kind="Internal").ap()
    r_xs = [Res("xs%d" % b) for b in range(NB)]
    r_qTd = [Res("qTd%d" % i) for i in range(NTL)]
    r_qiTd = [Res("qiTd%d" % i) for i in range(NTL)]
    r_oTd = [Res("oTd%d" % b) for b in range(NB)]
    r_out = Res("out")

    with ExitStack() as gst:
        P = Prog(nc, gst)

        uid = [0]

        def sb(st, name, shape, dt):
            uid[0] += 1
            return st.enter_context(nc.sbuf_tensor("%s_u%d" % (name, uid[0]), list(shape), dt))

        def ps(name, shape, dt):
            return gst.enter_context(nc.psum_tensor(name, list(shape), dt))

        pA = [ps("pA%d" % i, [128, 512], F32) for i in range(2)]
        pB = [ps("pB%d" % i, [128, 512], F32) for i in range(2)]
        pD = [ps("pD%d" % i, [128, 512], F32) for i in range(2)]
        pT = ps("pT0", [128, 1024], BF16)
        pX = ps("pX0", [128, 512], F32)
        r_pA = [Res("pA0"), Res("pA1")]
        r_pB = [Res("pB0"), Res("pB1")]
        r_pD = [Res("pD0"), Res("pD1")]
        r_pT = Res("pT")
        r_pX = Res("pX")
        ident = sb(gst, "ident_sb", [128, 128], BF16)
        ss_all = sb(gst, "ss_all", [128, NB], F32)
        rstd_all = sb(gst, "rstd_all", [128, NB], F32)
        r_ident, r_ss, r_rstd = Res("ident"), Res("ss"), Res("rstd")

        def mm(out, lhsT, rhs, start, stop, reads, writes, signal=None):
            P.op("pe", lambda h: h.matmul(out, lhsT=lhsT, rhs=rhs, start=start, stop=stop), reads, writes,
                 signal=(stop if signal is None else signal))

        def tr(out, in_, reads, writes, signal=True):
            P.op("pe", lambda h: h.transpose(out=out, in_=in_, identity=ident[:, :]), list(reads) + [r_ident], writes,
                 signal=signal)

        def act(out, in_, func, reads, writes, scale=1.0, bias=None, accum=None):
            kw = {}
            if bias is not None:
                kw["bias"] = bias
            if accum is not None:
                kw["accum_out"] = accum
            P.op("act", lambda h: h.activation(out=out, in_=in_, func=func, scale=scale, **kw), reads, writes)

        def ts(en, out, in0, s1, op0, reads, writes, s2=None, op1=None, accum=None):
            def f(h):
                kw = {}
                if accum is not None:
                    kw["accum_out"] = accum
                if op1 is None:
                    return h.tensor_scalar(out=out, in0=in0, scalar1=s1, scalar2=None, op0=op0, **kw)
                return h.tensor_scalar(out=out, in0=in0, scalar1=s1, scalar2=s2, op0=op0, op1=op1, **kw)
            P.op(en, f, reads, writes)

        def tt(en, out, in0, in1, op, reads, writes):
            P.op(en, lambda h: h.tensor_tensor(out=out, in0=in0, in1=in1, op=op), reads, writes)

        def stt(out, in0, scalar, in1, op0, op1, reads, writes, accum=None):
            if accum is None:
                P.op("dve", lambda h: h.scalar_tensor_tensor(out=out, in0=in0, scalar=scalar, in1=in1,
                                                             op0=op0, op1=op1), reads, writes)
            else:
                P.op("dve", lambda h: h.scalar_tensor_tensor(out=out, in0=in0, scalar=scalar, in1=in1,
                                                             op0=op0, op1=op1, accum_out=accum), reads, writes)

        def cp(en, out, in_, reads, writes):
            P.op(en, lambda h: h.tensor_copy(out=out, in_=in_), reads, writes)

        P.dma("sp", [(ident[:, :], ident_d)], writes=[r_ident], semres=r_ident)

        def load_w(st, name, src2d, kch, cols, csplit, eng="pool"):
            t = sb(st, name, [128, kch, cols], BF16)
            v = src2d.rearrange("(k p) f -> p k f", p=128)
            rs = []
            for c0 in range(0, cols, csplit):
                c1 = min(cols, c0 + csplit)
                r = Res("%s_%d" % (name, c0))
                P.dma(eng, [(t[:, :, c0:c1], v[:, :, c0:c1])], writes=[r], semres=r)
                rs.append(r)
            return t, rs, csplit

        def compute_rstd():
            act(rstd_all[:, :], ss_all[:, :], AF.Sqrt, [r_ss], [r_rstd], scale=1.0 / D, bias=EPS)
            P.op("dve", lambda h: h.reciprocal(out=rstd_all[:, :], in_=rstd_all[:, :]), [r_rstd], [r_rstd])

        class NormCtx:
            def __init__(self, st, g_row, xsrc):
                self.xsrc = xsrc
                self.gb = sb(st, "gb", [128, D], F32)
                self.r_gb = Res("gb")
                P.dma("sp", [(self.gb[:, :], nrm_d[g_row:g_row + 1, :].broadcast_to([128, D]))],
                      writes=[self.r_gb], semres=self.r_gb)
                self.xin = [sb(st, "xin%d" % i, [128, D], F32) for i in range(2)]
                self.r_xin = [Res("xin0"), Res("xin1")]
                self.hb = [sb(st, "hb%d" % i, [128, D], BF16) for i in range(2)]
                self.r_hb = [Res("hb0"), Res("hb1")]
                self.hT = sb(st, "hT", [128, KD, 512], BF16)
                self.r_hT = [Res("hT%d" % s) for s in range(4)]
                self.n = 0

            def tile(self, i, src_res=None):
                for s in range(4):
                    blk = 4 * i + s
                    sl = self.n % 2
                    self.n += 1
                    rd = [src_res[blk]] if src_res is not None else []
                    P.dma("sp", [(self.xin[sl][:, :], self.xsrc[blk * 128:(blk + 1) * 128, :])],
                          reads=rd, writes=[self.r_xin[sl]], semres=self.r_xin[sl])
                    stt(self.hb[sl][:, :], self.xin[sl][:, :], rstd_all[:, blk:blk + 1], self.gb[:, :],
                        ALU.mult, ALU.mult, [self.r_xin[sl], r_rstd, self.r_gb], [self.r_hb[sl]])
                    for k in range(KD):
                        tr(pT[:, k * 128:(k + 1) * 128], self.hb[sl][:, k * 128:(k + 1) * 128],
                           [self.r_hb[sl]], [r_pT], signal=(k == KD - 1))
                    act(self.hT[:, :, s * 128:(s + 1) * 128], pT[:, :].rearrange("p (k t) -> p k t", k=KD),
                        AF.Copy, [r_pT], [self.r_hT[s]])

        if True:
            xin = [sb(gst, "pxin%d" % i, [128, D], F32) for i in range(2)]
            r_xin = [Res("pxin0"), Res("pxin1")]
            junk = sb(gst, "pjunk", [128, D], BF16)
            r_junk = Res("pjunk")

            def prologue_stats():
                for blk in range(NB):
                    sl = blk % 2
                    P.dma("sp", [(xin[sl][:, :], x_d[blk * 128:(blk + 1) * 128, :])], writes=[r_xin[sl]], semres=r_xin[sl])
                    stt(junk[:, :], xin[sl][:, :], 1.0, xin[sl][:, :], ALU.mult, ALU.mult,
                        [r_xin[sl]], [r_junk, r_ss], accum=ss_all[:, blk:blk + 1])

        def ffn_phase(l, which, xsrc, src_res, first=False):
            with ExitStack() as st:
                g_row = l * 3 + (0 if which == 0 else 2)
                wgu, wgu_r, gsp = load_w(st, "wgu", wgu_d[which][l], KD, 2 * DFF, 512)
                wdn, wdn_r, dsp = load_w(st, "wdn", wdn_d[which][l], NF, D, 512)
                if first:
                    prologue_stats()
                compute_rstd()
                ncx = NormCtx(st, g_row, xsrc)
                actT = sb(st, "actT", [128, NF, 512], BF16)
                r_actT = [Res("actT%d" % f) for f in range(NF)]
                sg = [sb(st, "sg%d" % i, [128, 512], BF16) for i in range(2)]
                r_sg = [Res("sg0"), Res("sg1")]
                xr = [sb(st, "xr%d" % i, [128, D], F32) for i in range(2)]
                r_xr = [Res("xr0"), Res("xr1")]
                junk = sb(st, "junk", [128, D], BF16)
                r_junk = Res("junk")
                nxr = 0
                for i in range(NTL):
                    ncx.tile(i, src_res)
                    for f in range(NF):
                        sl = f % 2
                        cg = f * 128
                        cu = DFF + f * 128
                        for k in range(KD):
                            mm(pA[sl][:, :], wgu[:, k, cg:cg + 128], ncx.hT[:, k, :], k == 0, k == KD - 1,
                               ncx.r_hT + [wgu_r[cg // gsp]], [r_pA[sl]])
                        for k in range(KD):
                            mm(pB[sl][:, :], wgu[:, k, cu:cu + 128], ncx.hT[:, k, :], k == 0, k == KD - 1,
                               ncx.r_hT + [wgu_r[cu // gsp]], [r_pB[sl]])
                        act(sg[sl][:, :], pA[sl][:, :], AF.Silu, [r_pA[sl]], [r_sg[sl]])
                        tt("dve", actT[:, f, :], sg[sl][:, :], pB[sl][:, :], ALU.mult,
                           [r_sg[sl], r_pB[sl]], [r_actT[f]])
                    for s in range(4):
                        blk = 4 * i + s
                        xs_ = nxr % 2
                        nxr += 1
                        rd = [src_res[blk]] if src_res is not None else []
                        P.dma("sp", [(xr[xs_][:, :], xsrc[blk * 128:(blk + 1) * 128, :])], reads=rd,
                              writes=[r_xr[xs_]], semres=r_xr[xs_])
                        for half in range(2):
                            for f in range(NF):
                                mm(pD[half][:, :], actT[:, f, s * 128:(s + 1) * 128],
                                   wdn[:, f, half * 512:(half + 1) * 512], f == 0, f == NF - 1,
                                   [r_actT[f], wdn_r[(half * 512) // dsp]], [r_pD[half]])
                            stt(xr[xs_][:, half * 512:(half + 1) * 512], pD[half][:, :], 0.5,
                                xr[xs_][:, half * 512:(half + 1) * 512], ALU.mult, ALU.add,
                                [r_pD[half], r_xr[xs_]], [r_xr[xs_]])
                        stt(junk[:, :], xr[xs_][:, :], 1.0, xr[xs_][:, :], ALU.mult, ALU.mult,
                            [r_xr[xs_]], [r_junk, r_ss], accum=ss_all[:, blk:blk + 1])
                        P.dma("sp", [(xs_d[blk * 128:(blk + 1) * 128, :], xr[xs_][:, :])], reads=[r_xr[xs_]],
                              writes=[r_xs[blk]], semres=r_xr[xs_])
                P.end_phase()

        def attn_phases(l):
            with ExitStack() as ast:
                KT = sb(ast, "KT", [128, 4, T], BF16)
                VA = sb(ast, "VA", [128, NB, NHEAD, 65], BF16)
                kiT = sb(ast, "kiT", [128, T], BF16)
                wi_all = sb(ast, "wi_all", [128, NB, 8], F32)
                r_KT = [Res("KT%d" % i) for i in range(NTL)]
                r_VA = [Res("VA%d" % b) for b in range(NB)]
                r_kiT = [Res("kiT%d" % i) for i in range(NTL)]
                r_wi = [Res("wi%d" % b) for b in range(NB)]
                sgn_all = sb(ast, "sgn_all", [128, NB, 8], F32)
                absw_all = sb(ast, "absw_all", [128, NB, 8], F32)
                r_ones = Res("ones")
                with ExitStack() as st:
                    compute_rstd()
                    ncx = NormCtx(st, l * 3 + 1, xs_d)
                    P.op("pool", lambda h: h.memset(VA[:, :, :, 64:65], 1.0), [], [r_ones])
                    wA, wA_r, asp = load_w(st, "wA", wA_d[l], KD, WA_COLS, 512)
                    cs = sb(st, "cs", [128, 512], F32)
                    sn = sb(st, "sn", [128, 512], F32)
                    r_cs, r_sn = Res("cs"), Res("sn")
                    t1 = [sb(st, "t1_%d" % i, [128, 512], F32) for i in range(2)]
                    t2 = [sb(st, "t2_%d" % i, [128, 512], F32) for i in range(2)]
                    r_t1 = [Res("t1a"), Res("t1b")]
                    r_t2 = [Res("t2a"), Res("t2b")]
                    QTt = sb(st, "QTt", [128, 4, 512], BF16)
                    qiTt = sb(st, "qiTt", [128, 4, 512], BF16)
                    r_QTt, r_qiTt = Res("QTt"), Res("qiTt")
                    nrope = 0
                    for i in range(NTL):
                        ncx.tile(i, r_xs)
                        P.dma("sp", [(cs[:, :], cos_d[:, i * 512:(i + 1) * 512])], writes=[r_cs], semres=r_cs)
                        P.dma("sp", [(sn[:, :], sin_d[:, i * 512:(i + 1) * 512])], writes=[r_sn], semres=r_sn)

                        def rope(ca, cb, dest, r_dest):
                            nonlocal nrope
                            sl = nrope % 2
                            nrope += 1
                            for k in range(KD):
                                mm(pA[sl][:, :], wA[:, k, ca:ca + 128], ncx.hT[:, k, :], k == 0, k == KD - 1,
                                   ncx.r_hT + [wA_r[ca // asp]], [r_pA[sl]])
                            for k in range(KD):
                                mm(pB[sl][:, :], wA[:, k, cb:cb + 128], ncx.hT[:, k, :], k == 0, k == KD - 1,
                                   ncx.r_hT + [wA_r[cb // asp]], [r_pB[sl]])
                            tt("dve", t1[sl][:, :], pA[sl][:, :], cs[:, :], ALU.mult, [r_pA[sl], r_cs], [r_t1[sl]])
                            tt("dve", t2[sl][:, :], pB[sl][:, :], sn[:, :], ALU.mult, [r_pB[sl], r_sn], [r_t2[sl]])
                            tt("pool", dest, t1[sl][:, :], t2[sl][:, :], ALU.add, [r_t1[sl], r_t2[sl]], [r_dest])
                        for c in range(4):
                            rope(c * 128, 512 + c * 128, QTt[:, c, :], r_QTt)
                        P.dma("sp", [(qT_d[:, :, i * 512:(i + 1) * 512], QTt[:, :, :])], reads=[r_QTt],
                              writes=[r_qTd[i]], semres=r_QTt)
                        for c in range(4):
                            rope(1024 + c * 128, 1536 + c * 128, KT[:, c, i * 512:(i + 1) * 512], r_KT[i])
                        for c in range(4):
                            rope(2048 + c * 128, 2560 + c * 128, qiTt[:, c, :], r_qiTt)
                        P.dma("sp", [(qiT_d[:, :, i * 512:(i + 1) * 512], qiTt[:, :, :])], reads=[r_qiTt],
                              writes=[r_qiTd[i]], semres=r_qiTt)
                        rope(3072, 3200, kiT[:, i * 512:(i + 1) * 512], r_kiT[i])
                        for s in range(4):
                            blk = 4 * i + s
                            hf = s % 2
                            for k in range(KD):
                                mm(pD[hf][:, :], ncx.hT[:, k, s * 128:(s + 1) * 128], wA[:, k, 3328:3840],
                                   k == 0, k == KD - 1, [ncx.r_hT[s], wA_r[3328 // asp], wA_r[3839 // asp]], [r_pD[hf]])
                            act(VA[:, blk, :, 0:64], pD[hf][:, :].rearrange("p (h d) -> p h d", h=NHEAD), AF.Copy,
                                [r_pD[hf], r_ones], [r_VA[blk]])
                            for k in range(KD):
                                mm(pX[:, 0:8], ncx.hT[:, k, s * 128:(s + 1) * 128], wA[:, k, 3840:3848],
                                   k == 0, k == KD - 1, [ncx.r_hT[s], wA_r[3840 // asp]], [r_pX])
                            ts("dve", wi_all[:, blk, :], pX[:, 0:8], IDX_SCALE, ALU.mult, [r_pX], [r_wi[blk]])
                    P.end_phase()
                if stop_after == "MA1":
                    return
                with ExitStack() as st:
                    QTt = [sb(st, "QTt%d" % i, [128, 4, 512], BF16) for i in range(2)]
                    qiTt = [sb(st, "qiTt%d" % i, [128, 4, 512], BF16) for i in range(2)]
                    r_QTt = [Res("QTt0"), Res("QTt1")]
                    r_qiTt = [Res("qiTt0"), Res("qiTt1")]
                    S = [sb(st, "S%d" % i, [128, T], F32) for i in range(2)]
                    r_S = [[Res("S%d_%d" % (i, c)) for c in range(NTL)] for i in range(2)]
                    rb = [sb(st, "rb%d" % i, [128, 512], BF16) for i in range(5)]
                    r_rb = [Res("rb%d" % i) for i in range(5)]
                    Dh = [sb(st, "Dh%d" % i, [128, 8, 128], BF16) for i in range(2)]
                    r_Dh = [Res("Dh0"), Res("Dh1")]
                    dmask = sb(st, "dmask", [128, 128], F32)
                    r_dmask = Res("dmask")
                    P.dma("sp", [(dmask[:, :], dmask_d)], writes=[r_dmask], semres=r_dmask)
                    junkb = sb(st, "junkb", [128, T], BF16)
                    r_junkb = Res("junkb")
                    sv = sb(st, "sv", [128, 8], F32)
                    r_sv = Res("sv")
                    pwt = sb(st, "pwt", [128, 48], F32)
                    r_pwt = Res("pwt")
                    P.dma("sp", [(pwt[:, :], pwt_d)], writes=[r_pwt], semres=r_pwt)
                    qtab = sb(st, "qtab", [128, 48], F32)
                    r_qtab = Res("qtab")
                    MB = sb(st, "MB", [128, T], BF16)
                    r_MB = Res("MB")
                    MBT = [sb(st, "MBT%d" % i, [128, NB, 128], BF16) for i in range(2)]
                    r_MBT = [Res("MBT0"), Res("MBT1")]
                    PT = [sb(st, "PT%d" % i, [128, 512], BF16) for i in range(5)]
                    r_PT = [Res("PT%d" % i) for i in range(5)]
                    PTm = [sb(st, "PTm%d" % i, [128, 512], BF16) for i in range(5)]
                    r_PTm = [Res("PTm%d" % i) for i in range(5)]
                    junka = sb(st, "junka", [128, T], BF16)
                    r_junka = Res("junka")
                    obr = [sb(st, "obr%d" % i, [128, NHEAD, 65], F32) for i in range(2)]
                    r_obr = [Res("obr0"), Res("obr1")]
                    ob = sb(st, "ob", [128, 512], BF16)
                    r_ob = Res("ob")
                    rc = sb(st, "rc", [128, 8], F32)
                    r_rc = Res("rc")
                    oTq = [sb(st, "oTq%d" % i, [128, 4, 128], BF16) for i in range(2)]
                    r_oTq = [Res("oTq0"), Res("oTq1")]
                    cnt_ = {"rl": 0, "pt": 0}

                    def stage_A1(qb):
                        i = qb // 4
                        tq = qb % 4
                        ts_ = i % 2
                        if tq == 0:
                            P.dma("sp", [(QTt[ts_][:, :, :], qT_d[:, :, i * 512:(i + 1) * 512])], reads=[r_qTd[i]],
                                  writes=[r_QTt[ts_]], semres=r_QTt[ts_])
                            P.dma("sp", [(qiTt[ts_][:, :, :], qiT_d[:, :, i * 512:(i + 1) * 512])], reads=[r_qiTd[i]],
                                  writes=[r_qiTt[ts_]], semres=r_qiTt[ts_])
                        L = (qb + 1) * 128
                        nch = (L + 511) // 512
                        ss_ = qb % 2
                        Sq = S[ss_]
                        ds_ = qb % 2
                        for h in range(8):
                            ts("dve", Dh[ds_][:, h, :], ident[:, :], wi_all[:, qb, h:h + 1], ALU.mult,
                               [r_ident, r_wi[qb]], [r_Dh[ds_]])
                        yield
                        pend = []

                        def emit_sum(item):
                            c, N, h, rs_ = item
                            mm(pX[:, 0:N], Dh[ds_][:, h, :], rb[rs_][:, 0:N], h == 0, h == 7,
                               [r_Dh[ds_], r_rb[rs_]], [r_pX], signal=True)
                            if h == 7:
                                c0 = c * 512
                                if c == nch - 1:
                                    if N > 128:
                                        cp("dve", Sq[:, c0:c0 + N - 128], pX[:, 0:N - 128], [r_pX], [r_S[ss_][c]])
                                    tt("dve", Sq[:, L - 128:L], pX[:, N - 128:N], dmask[:, :], ALU.add,
                                       [r_pX, r_dmask], [r_S[ss_][c]])
                                else:
                                    cp("dve", Sq[:, c0:c0 + N], pX[:, 0:N], [r_pX], [r_S[ss_][c]])
                        for c in range(nch):
                            N = min(512, L - 512 * c)
                            for h in range(8):
                                ph = 64 * (h % 2)
                                pb_ = h % 2
                                mm(pB[pb_][:, 0:N], qiTt[ts_][ph:ph + 64, h // 2, tq * 128:(tq + 1) * 128],
                                   kiT[ph:ph + 64, c * 512:c * 512 + N], True, True,
                                   [r_qiTt[ts_], r_kiT[c]], [r_pB[pb_]])
                                rs_ = cnt_["rl"] % 5
                                cnt_["rl"] += 1
                                act(rb[rs_][:, 0:N], pB[pb_][:, 0:N], AF.Relu, [r_pB[pb_]], [r_rb[rs_]])
                                pend.append((c, N, h, rs_))
                                while len(pend) > 2:
                                    emit_sum(pend.pop(0))
                                yield
                        while pend:
                            emit_sum(pend.pop(0))
                            yield

                    def stage_A2(qb):
                        L = (qb + 1) * 128
                        nch = (L + 511) // 512
                        ss_ = qb % 2
                        Sq = S[ss_]
                        rS = r_S[ss_][:nch]
                        if L > topk:
                            P.op("dve", lambda h: h.tensor_reduce(out=sv[:, 0:1], in_=Sq[:, 0:L - 128], axis=AX.X, op=ALU.max,
                                                                  apply_absolute_value=True), rS, [r_sv])
                            yield
                            ts("dve", sv[:, 2:3], sv[:, 0:1], 2.0, ALU.mult, [r_sv], [r_sv])
                            yield
                            ts("dve", qtab[:, :], pwt[:, :], sv[:, 2:3], ALU.mult, [r_pwt, r_sv], [r_qtab])
                            yield
                            ts("dve", sv[:, 3:4], sv[:, 0:1], 0.0, ALU.mult, [r_sv], [r_sv])
                            yield
                            for n in range(NIT):
                                if n % 5 == 2:
                                    act(junka[:, 0:L], Sq[:, 0:L], AF.Sign, rS + [r_sv], [r_junka, r_sv], scale=-1.0,
                                        bias=sv[:, 3:4], accum=sv[:, 4:5])
                                    yield
                                    ts("dve", sv[:, 5:6], sv[:, 4:5], float(L - 2 * topk) + 0.5, ALU.is_le, [r_sv, r_qtab], [r_sv],
                                       s2=qtab[:, 24 + n:25 + n], op1=ALU.mult)
                                    yield
                                else:
                                    ts("dve", junkb[:, 0:L], Sq[:, 0:L], sv[:, 3:4], ALU.is_ge, rS + [r_sv], [r_junkb, r_sv],
                                       s2=0.0, op1=ALU.add, accum=sv[:, 4:5])
                                    yield
                                    ts("dve", sv[:, 5:6], sv[:, 4:5], float(topk) - 0.5, ALU.is_ge, [r_sv, r_qtab], [r_sv],
                                       s2=qtab[:, 24 + n:25 + n], op1=ALU.mult)
                                    yield
                                stt(sv[:, 3:4], sv[:, 5:6], qtab[:, n:n + 1], sv[:, 3:4], ALU.subtract, ALU.add,
                                    [r_sv, r_qtab], [r_sv])
                                yield
                            tt("dve", sv[:, 1:2], sv[:, 3:4], qtab[:, 24 + NIT:25 + NIT], ALU.subtract, [r_sv, r_qtab], [r_sv])
                            yield
                        else:
                            P.op("dve", lambda h: h.memset(sv[:, 1:2], -1e29), [], [r_sv])
                            yield

                    def stage_A3(qb):
                        L = (qb + 1) * 128
                        nch = (L + 511) // 512
                        ss_ = qb % 2
                        Sq = S[ss_]
                        rS = r_S[ss_][:nch]
                        ts("dve", MB[:, 0:L], Sq[:, 0:L], sv[:, 1:2], ALU.is_ge, rS + [r_sv], [r_MB])
                        ms = qb % 2
                        for j0 in range(0, qb + 1, 8):
                            nj = min(8, qb + 1 - j0)
                            for jj in range(nj):
                                j = j0 + jj
                                tr(pT[:, jj * 128:(jj + 1) * 128], MB[:, j * 128:(j + 1) * 128], [r_MB], [r_pT],
                                   signal=(jj == nj - 1))
                            cp("dve", MBT[ms][:, j0:j0 + nj, :], pT[:, 0:nj * 128].rearrange("p (j t) -> p j t", j=nj),
                               [r_pT], [r_MBT[ms]])

                    def stage_B(qb):
                        i = qb // 4
                        tq = qb % 4
                        ts_ = i % 2
                        ms = qb % 2
                        os_ = qb % 2
                        pend = []

                        def emit_pv(item):
                            kind = item[0]
                            if kind == "pv":
                                _, h, jg, nj, pts = item
                                po = pD[h % 2]
                                for jj in range(nj):
                                    j = jg + jj
                                    mm(po[:, 0:65], PTm[pts][:, jj * 128:(jj + 1) * 128], VA[:, j, h, :],
                                       j == 0, j == qb, [r_PTm[pts], r_VA[j], r_ones], [r_pD[h % 2]],
                                       signal=(jj == nj - 1))
                            else:
                                _, h = item
                                act(obr[os_][:, h, :], pD[h % 2][:, 0:65], AF.Copy, [r_pD[h % 2]], [r_obr[os_]])
                        for h in range(NHEAD):
                            ph = 64 * (h % 2)
                            for jg in range(0, qb + 1, 4):
                                nj = min(4, qb + 1 - jg)
                                pa_ = cnt_["pt"] % 2
                                for jj in range(nj):
                                    j = jg + jj
                                    mm(pA[pa_][:, jj * 128:(jj + 1) * 128], KT[ph:ph + 64, h // 2, j * 128:(j + 1) * 128],
                                       QTt[ts_][ph:ph + 64, h // 2, tq * 128:(tq + 1) * 128], True, True,
                                       [r_KT[j // 4], r_QTt[ts_]], [r_pA[pa_]], signal=(jj == nj - 1))
                                pts = cnt_["pt"] % 5
                                cnt_["pt"] += 1
                                act(PT[pts][:, 0:nj * 128], pA[pa_][:, 0:nj * 128], AF.Exp, [r_pA[pa_]], [r_PT[pts]],
                                    scale=ATT_SCALE)
                                tt("pool", PTm[pts][:, 0:nj * 128], PT[pts][:, 0:nj * 128],
                                   MBT[ms][:, jg:jg + nj, :].rearrange("p j t -> p (j t)"), ALU.mult,
                                   [r_PT[pts], r_MBT[ms]], [r_PTm[pts]])
                                pend.append(("pv", h, jg, nj, pts))
                                if jg + 4 > qb:
                                    pend.append(("evac", h))
                                while len([p for p in pend if p[0] == "pv"]) > 2:
                                    emit_pv(pend.pop(0))
                                    while pend and pend[0][0] == "evac":
                                        emit_pv(pend.pop(0))
                                yield
                        while pend:
                            emit_pv(pend.pop(0))
                            yield

                    def stage_C(qb):
                        os_ = qb % 2
                        P.op("dve", lambda h_: h_.reciprocal(out=rc[:, :], in_=obr[os_][:, :, 64]), [r_obr[os_]], [r_rc])
                        for h in range(NHEAD):
                            ts("dve", ob[:, h * 64:(h + 1) * 64], obr[os_][:, h, 0:64], rc[:, h:h + 1], ALU.mult,
                               [r_obr[os_], r_rc], [r_ob])
                        for c in range(4):
                            tr(pT[:, c * 128:(c + 1) * 128], ob[:, c * 128:(c + 1) * 128], [r_ob], [r_pT],
                               signal=(c == 3))
                        cp("dve", oTq[os_][:, :, :], pT[:, 0:512].rearrange("p (c t) -> p c t", c=4), [r_pT], [r_oTq[os_]])
                        P.dma("sp", [(oT_d[:, :, qb * 128:(qb + 1) * 128], oTq[os_][:, :, :])], reads=[r_oTq[os_]],
                              writes=[r_oTd[qb]], semres=r_oTq[os_])

                    def run_all(gens):
                        gens = [g for g in gens if g is not None]
                        while gens:
                            for g in list(gens):
                                try:
                                    next(g)
                                except StopIteration:
                                    gens.remove(g)

                    run_all([stage_A1(0)])
                    for qb in range(NB):
                        run_all([stage_A2(qb),
                                 stage_A1(qb + 1) if qb + 1 < NB else None,
                                 stage_B(qb - 1) if qb >= 1 else None])
                        stage_A3(qb)
                        if qb >= 1:
                            stage_C(qb - 1)
                    run_all([stage_B(NB - 1)])
                    stage_C(NB - 1)
                    P.end_phase()

        def mb_phase(l):
            with ExitStack() as st:
                ncx = NormCtx(st, l * 3 + 1, xs_d)
                wB, wB_r, bsp = load_w(st, "wB", wB_d[l], KD, WB_COLS, 512)
                wpa, wpa_r, _ = load_w(st, "wpa", wpa_d[l], 4, D, 1024)
                wpb, wpb_r, _ = load_w(st, "wpb", wpb_d[l], 4, D, 1024)
                wout, wout_r, _ = load_w(st, "wout", wout_d[l], KD, D, 1024)
                bgT = sb(st, "bgT", [128, 16], F32)
                lng = sb(st, "lng", [128, 512], F32)
                lnb = sb(st, "lnb", [128, 512], F32)
                bsb = sb(st, "bsb", [128, 512], F32)
                wsf = sb(st, "wsf", [128, 512], F32)
                trl = sb(st, "trl", [128, 512], F32)
                wsT = sb(st, "wsT", [128, 512], BF16)
                r_c = Res("mbconst")
                r_wsf, r_trl, r_wsT = Res("wsf"), Res("trl"), Res("wsT")
                P.dma("sp", [(bgT[:, :], bgT_d[l]),
                             (lng[:, :], lng_d[l:l + 1, :].broadcast_to([128, 512])),
                             (lnb[:, :], lnb_d[l:l + 1, :].broadcast_to([128, 512])),
                             (bsb[:, :], bsb_d[l:l + 1, :].broadcast_to([128, 512]))], writes=[r_c], semres=r_c)
                P.dma("sp", [(wsf[:, :], wsT_d[l])], writes=[r_wsf], semres=r_wsf)
                P.dma("sp", [(trl[:, :], tril_d)], writes=[r_trl], semres=r_trl)
                tt("dve", wsT[:, :], wsf[:, :], trl[:, :], ALU.mult, [r_wsf, r_trl], [r_wsT])
                guT = sb(st, "guT", [128, 4, 512], BF16)
                r_guT = [Res("guT%d" % c) for c in range(4)]
                gT = sb(st, "gT", [128, 16, 512], BF16)
                r_gT = [Res("gT%d" % c) for c in range(16)]
                gv = [sb(st, "gv%d" % i, [128, 512], F32) for i in range(4)]
                r_gv = [Res("gv%d" % i) for i in range(4)]
                vb = [sb(st, "vb%d" % i, [128, 512], BF16) for i in range(4)]
                r_vb = [Res("vb%d" % i) for i in range(4)]
                st6 = sb(st, "st6", [128, 4, 6], F32)
                mv = sb(st, "mv", [128, 4, 2], F32)
                lr = sb(st, "lr", [128, 4], F32)
                r_st6 = [Res("st6_%d" % i) for i in range(4)]
                r_mv = [Res("mv%d" % i) for i in range(4)]
                r_lr = [Res("lr%d" % i) for i in range(4)]
                negh = sb(st, "negh", [128, 2], F32)
                r_negh = Res("negh")
                P.op("pool", lambda h: h.memset(negh[:, :], -0.5), [], [r_negh])
                yapT = sb(st, "yapT", [128, 4, 512], BF16)
                r_yap = [Res("yap%d" % s) for s in range(4)]
                tmpf = [sb(st, "tmpf%d" % i, [128, 512], F32) for i in range(2)]
                r_tmpf = [Res("tmpf0"), Res("tmpf1")]
                tmpg = [sb(st, "tmpg%d" % i, [128, 512], F32) for i in range(2)]
                r_tmpg = [Res("tmpg0"), Res("tmpg1")]
                oTt = [sb(st, "oTt%d" % i, [128, 4, 512], BF16) for i in range(2)]
                r_oTt = [Res("oTt0"), Res("oTt1")]
                zT = sb(st, "zT", [128, 8, 512], BF16)
                r_zT = [Res("zT%d" % m) for m in range(8)]
                xr = [sb(st, "xr%d" % i, [128, D], F32) for i in range(2)]
                r_xr = [Res("xr0"), Res("xr1")]
                junk = sb(st, "junk", [128, D], BF16)
                r_junk = Res("junk")
                nxr = 0
                ngv = 0
                ntm = 0
                for i in range(NTL):
                    ncx.tile(i, r_xs)
                    os_ = i % 2
                    P.dma("sp", [(oTt[os_][:, :, :], oT_d[:, :, i * 512:(i + 1) * 512])],
                          reads=r_oTd[4 * i:4 * i + 4], writes=[r_oTt[os_]], semres=r_oTt[os_])
                    for c in range(4):
                        sl = c % 2
                        for k in range(KD):
                            mm(pA[sl][:, :], wB[:, k, c * 128:(c + 1) * 128], ncx.hT[:, k, :], k == 0, k == KD - 1,
                               ncx.r_hT + [wB_r[(c * 128) // bsp]], [r_pA[sl]])
                        act(guT[:, c, :], pA[sl][:, :], AF.Gelu_apprx_tanh, [r_pA[sl]], [r_guT[c]])
                    for s in range(4):
                        hf = s % 2
                        for k in range(KD):
                            mm(pD[hf][:, :], ncx.hT[:, k, s * 128:(s + 1) * 128], wB[:, k, 2560:3072], k == 0, k == KD - 1,
                               [ncx.r_hT[s], wB_r[2560 // bsp]], [r_pD[hf]])
                        g_ = s
                        ngv += 1
                        act(gv[g_][:, :], pD[hf][:, :], AF.Gelu_apprx_tanh, [r_pD[hf]], [r_gv[g_]])
                        P.op("dve", lambda h, g_=g_: h.bn_stats(out=st6[:, g_, :], in_=gv[g_][:, :]), [r_gv[g_]], [r_st6[g_]])
                        P.op("dve", lambda h, g_=g_: h.bn_aggr(out=mv[:, g_, :], in_=st6[:, g_, :]), [r_st6[g_]], [r_mv[g_]])
                        ts("dve", lr[:, g_:g_ + 1], mv[:, g_, 1:2], EPS, ALU.add, [r_mv[g_]], [r_lr[g_]])
                        tt("pool", lr[:, g_:g_ + 1], lr[:, g_:g_ + 1], negh[:, 0:1], ALU.pow, [r_lr[g_], r_negh], [r_lr[g_]])
                        ts("dve", gv[g_][:, :], gv[g_][:, :], mv[:, g_, 0:1], ALU.subtract, [r_gv[g_], r_mv[g_], r_lr[g_]],
                           [r_gv[g_]], s2=lr[:, g_:g_ + 1], op1=ALU.mult)
                        tt("dve", gv[g_][:, :], gv[g_][:, :], lng[:, :], ALU.mult, [r_gv[g_], r_c], [r_gv[g_]])
                        tt("dve", vb[g_][:, :], gv[g_][:, :], lnb[:, :], ALU.add, [r_gv[g_], r_c], [r_vb[g_]])
                    for c in range(16):
                        sl = c % 2
                        c0 = 512 + c * 128
                        for k in range(KD):
                            mm(pB[sl][:, :], wB[:, k, c0:c0 + 128], ncx.hT[:, k, :], k == 0, k == KD - 1,
                               ncx.r_hT + [wB_r[c0 // bsp]], [r_pB[sl]])
                        act(gT[:, c, :], pB[sl][:, :], AF.Sigmoid, [r_pB[sl], r_c], [r_gT[c]], bias=bgT[:, c:c + 1])
                    for s in range(4):
                        g_ = s
                        for g in range(4):
                            mm(pX[:, g * 128:(g + 1) * 128], vb[g_][:, g * 128:(g + 1) * 128], wsT[:, g * 128:(g + 1) * 128],
                               True, True, [r_vb[g_], r_wsT], [r_pX])
                        t_ = ntm % 2
                        ntm += 1
                        tt("dve", tmpf[t_][:, :], pX[:, :], bsb[:, :], ALU.add, [r_pX, r_c], [r_tmpf[t_]])
                        tt("dve", yapT[:, :, s * 128:(s + 1) * 128], tmpf[t_][:, :].rearrange("p (g t) -> p g t", g=4),
                           guT[:, :, s * 128:(s + 1) * 128], ALU.mult, [r_tmpf[t_]] + r_guT, [r_yap[s]])
                    for m in range(8):
                        sl = m % 2
                        for g in range(4):
                            mm(pA[sl][:, :], wpa[:, g, m * 128:(m + 1) * 128], yapT[:, g, :], g == 0, g == 3,
                               r_yap + wpa_r, [r_pA[sl]])
                        for c in range(4):
                            mm(pB[sl][:, :], wpb[:, c, m * 128:(m + 1) * 128], oTt[os_][:, c, :], c == 0, c == 3,
                               [r_oTt[os_]] + wpb_r, [r_pB[sl]])
                        tt("dve", tmpf[sl][:, :], pA[sl][:, :], gT[:, m, :], ALU.mult, [r_pA[sl], r_gT[m]], [r_tmpf[sl]])
                        tt("dve", tmpg[sl][:, :], pB[sl][:, :], gT[:, 8 + m, :], ALU.mult, [r_pB[sl], r_gT[8 + m]], [r_tmpg[sl]])
                        tt("pool", zT[:, m, :], tmpf[sl][:, :], tmpg[sl][:, :], ALU.add, [r_tmpf[sl], r_tmpg[sl]], [r_zT[m]])
                    for s in range(4):
                        blk = 4 * i + s
                        xs_ = nxr % 2
                        nxr += 1
                        P.dma("sp", [(xr[xs_][:, :], xs_d[blk * 128:(blk + 1) * 128, :])], reads=[r_xs[blk]],
                              writes=[r_xr[xs_]], semres=r_xr[xs_])
                        for half in range(2):
                            for j in range(8):
                                mm(pD[half][:, :], zT[:, j, s * 128:(s + 1) * 128], wout[:, j, half * 512:(half + 1) * 512],
                                   j == 0, j == 7, [r_zT[j]] + wout_r, [r_pD[half]])
                            tt("dve", xr[xs_][:, half * 512:(half + 1) * 512], pD[half][:, :],
                               xr[xs_][:, half * 512:(half + 1) * 512], ALU.add, [r_pD[half], r_xr[xs_]], [r_xr[xs_]])
                        stt(junk[:, :], xr[xs_][:, :], 1.0, xr[xs_][:, :], ALU.mult, ALU.mult,
                            [r_xr[xs_]], [r_junk, r_ss], accum=ss_all[:, blk:blk + 1])
                        P.dma("sp", [(xs_d[blk * 128:(blk + 1) * 128, :], xr[xs_][:, :])], reads=[r_xr[xs_]],
                              writes=[r_xs[blk]], semres=r_xr[xs_])
                P.end_phase()

        def final_phase(src, src_res):
            with ExitStack() as st:
                compute_rstd()
                gb = sb(st, "fgb", [128, D], F32)
                r_gb = Res("fgb")
                P.dma("sp", [(gb[:, :], nrm_d[DEPTH * 3:DEPTH * 3 + 1, :].broadcast_to([128, D]))], writes=[r_gb], semres=r_gb)
                xr = [sb(st, "fxr%d" % i, [128, D], F32) for i in range(2)]
                r_xr = [Res("fxr0"), Res("fxr1")]
                toks = []
                for blk in range(NB):
                    sl = blk % 2
                    rd = [src_res[blk]] if src_res is not None else []
                    P.dma("sp", [(xr[sl][:, :], src[blk * 128:(blk + 1) * 128, :])], reads=rd, writes=[r_xr[sl]], semres=r_xr[sl])
                    stt(xr[sl][:, :], xr[sl][:, :], rstd_all[:, blk:blk + 1], gb[:, :], ALU.mult, ALU.mult,
                        [r_xr[sl], r_rstd, r_gb], [r_xr[sl]])
                    P.dma("sp", [(out_d[blk * 128:(blk + 1) * 128, :], xr[sl][:, :])], reads=[r_xr[sl]], writes=[r_out], semres=r_xr[sl])
                P.end_phase()

        done = False
        src, src_res = x_d, None
        for l in range(DEPTH):
            ffn_phase(l, 0, src, src_res, first=(l == 0))
            src, src_res = xs_d, r_xs
            if stop_after == "F1":
                done = True
                break
            attn_phases(l)
            if stop_after in ("MA1", "MA2"):
                done = True
                break
            mb_phase(l)
            if stop_after == "MB":
                done = True
                break
            ffn_phase(l, 1, xs_d, r_xs)
        if stop_after is None or stop_after in ("F1", "MB"):
            final_phase(src, src_res)
        else:
            final_phase(x_d, None)
        build.info = dict(ninst=P.ninst, nsem=P.nsem, nblocks=P.nblocks)
    return nc


def _swap_cols(n_heads, hd=64):
    idx = []
    for h in range(n_heads):
        idx += list(range(h * hd + hd // 2, (h + 1) * hd)) + list(range(h * hd, h * hd + hd // 2))
    return np.array(idx)


def prep_shared(inp, T, DEPTH, DFF):
    f32 = np.float32
    w_in = np.asarray(inp["w_in"], f32)
    o = {}
    u0, va0, q0, k0, v0, qi0, ki0, wi0, ga0, gb0 = 0, 512, 1024, 1536, 2048, 2560, 3072, 3136, 3144, 4168
    sw8 = _swap_cols(8)
    sw1 = _swap_cols(1)
    q = w_in[:, :, q0:q0 + 512]
    k = w_in[:, :, k0:k0 + 512]
    qi = w_in[:, :, qi0:qi0 + 512]
    ki = w_in[:, :, ki0:ki0 + 64]
    ki_sw = ki[:, :, sw1]
    wA = np.concatenate([q, q[:, :, sw8], k, k[:, :, sw8], qi, qi[:, :, sw8], ki, ki, ki_sw, ki_sw,
                         w_in[:, :, v0:v0 + 512], w_in[:, :, wi0:wi0 + 8]], axis=2)
    assert wA.shape[2] == WA_COLS
    wB = np.concatenate([w_in[:, :, u0:u0 + 512], w_in[:, :, ga0:ga0 + 1024], w_in[:, :, gb0:gb0 + 1024],
                         w_in[:, :, va0:va0 + 512]], axis=2)
    assert wB.shape[2] == WB_COLS
    o["wA"] = np.ascontiguousarray(wA)
    o["wB"] = np.ascontiguousarray(wB)
    nrm = np.stack([np.asarray(inp["ffn1_norm"], f32), np.asarray(inp["mix_norm"], f32),
                    np.asarray(inp["ffn2_norm"], f32)], axis=1).reshape(DEPTH * 3, D)
    o["nrm"] = np.ascontiguousarray(np.concatenate([nrm, np.asarray(inp["final_norm"], f32)[None, :]], axis=0))
    o["wgu1"] = np.ascontiguousarray(inp["ffn1_w_gu"], f32)
    o["wgu2"] = np.ascontiguousarray(inp["ffn2_w_gu"], f32)
    o["wdn1"] = np.ascontiguousarray(inp["ffn1_w_down"], f32)
    o["wdn2"] = np.ascontiguousarray(inp["ffn2_w_down"], f32)
    bg = np.asarray(inp["b_gate"], f32)
    o["bgT"] = np.ascontiguousarray(bg.reshape(DEPTH, 16, 128).transpose(0, 2, 1))
    o["lng"] = np.ascontiguousarray(inp["gmlp_ln_g"], f32)
    o["lnb"] = np.ascontiguousarray(inp["gmlp_ln_b"], f32)
    ws = np.asarray(inp["gmlp_w_s"], f32)
    o["wsT"] = np.ascontiguousarray(ws.transpose(0, 3, 1, 2).reshape(DEPTH, 128, 512))
    o["bsb"] = np.ascontiguousarray(np.asarray(inp["gmlp_b_s"], f32).reshape(DEPTH, 512))
    o["wpa"] = np.ascontiguousarray(inp["w_pa"], f32)
    o["wpb"] = np.ascontiguousarray(inp["w_pb"], f32)
    o["wout"] = np.ascontiguousarray(inp["w_out"], f32)
    o["ident"] = np.eye(128, dtype=f32).astype(ml_dtypes.bfloat16)
    inv = (np.float32(10000.0) ** (-np.arange(0, 64, 2, dtype=f32) / np.float32(64))).astype(f32)
    ang = np.arange(T, dtype=f32)[:, None] * inv[None, :]
    cos = np.cos(ang).astype(f32).T
    sin = np.sin(ang).astype(f32).T
    cos64 = np.concatenate([cos, cos], axis=0)
    sin64 = np.concatenate([-sin, sin], axis=0)
    o["cosT"] = np.ascontiguousarray(np.concatenate([cos64, cos64], axis=0))
    o["sinT"] = np.ascontiguousarray(np.concatenate([sin64, sin64], axis=0))
    tq = np.arange(128)
    o["dmask"] = np.where(tq[None, :] <= tq[:, None], 0.0, -1e30).astype(f32)
    tr1 = (tq[:, None] <= tq[None, :]).astype(f32)
    o["tril"] = np.ascontiguousarray(np.tile(tr1, (1, 4)))
    pw = np.concatenate([2.0 ** -(np.arange(24) + 2.0), 2.0 ** -(np.arange(24) + 1.0)]).astype(f32)
    o["pwt"] = np.ascontiguousarray(np.tile(pw[None, :], (128, 1)))
    return o


_T, _DEPTH, _DFF = 4096, 2, 2816


def kernel(**inputs):
    x = np.asarray(inputs["x"], np.float32)
    B = x.shape[0]
    shared = prep_shared(inputs, _T, _DEPTH, _DFF)
    nc = build(_T, _DEPTH, _DFF)
    in_maps = []
    for b in range(B):
        m = dict(shared)
        m["x"] = np.ascontiguousarray(x[b])
        in_maps.append(m)
    res = run_bass_kernel_spmd(nc, in_maps, core_ids=list(range(B)))
    return np.stack([np.asarray(r["out"], np.float32) for r in res.results], axis=0)
```
